# Optimizing a Trainium2 kernel written in Bass

```python
import jax, jax.numpy as jnp
from jax import lax
import numpy as np

D_MODEL = 1024
BATCH = 8
SEQ = 8192
DEPTH = 1

N_META = 16
EPS = 1e-6

SB_HEADS = 8
SB_HEAD_DIM = D_MODEL // 16
SB_WIDTH = SB_HEADS * SB_HEAD_DIM
SB_BLOCK = 128

GLA_HEADS = 4
GLA_DV = D_MODEL // 8
GLA_DK = GLA_DV // 2
GLA_WIDTH = GLA_HEADS * GLA_DV
GLA_KW = GLA_HEADS * GLA_DK
GLA_GATE_RANK = 16
GLA_TAU = 16.0
GLA_CHUNK = 64

MIX_WIDTH = SB_WIDTH + GLA_WIDTH
IN_SPLITS = [SB_WIDTH, SB_WIDTH, SB_WIDTH, GLA_KW, GLA_KW, GLA_WIDTH, GLA_WIDTH, GLA_GATE_RANK]
IN_COLS = sum(IN_SPLITS)

PEER_HEADS = 8
PEER_NKEYS = 128
PEER_TOPK = 16
PEER_QDIM = 128
PEER_HALF = PEER_QDIM // 2
N_EXPERTS = PEER_NKEYS * PEER_NKEYS
PEER_BLOCK = 256

kernel_name = "hymba_sb_gla_peer_block"


def rmsnorm(x, g):
    xf = x.astype(jnp.float32)
    y = xf * lax.rsqrt(jnp.mean(xf * xf, axis=-1, keepdims=True) + EPS)
    return (y * g.astype(jnp.float32)).astype(x.dtype)


def split_heads(t, n):
    b, l, _ = t.shape
    return t.reshape(b, l, n, -1).transpose(0, 2, 1, 3)


def merge_heads(t):
    b, h, l, d = t.shape
    return t.transpose(0, 2, 1, 3).reshape(b, l, h * d)


def sb_query_block(q_blk, q_pos, k, v):
    L = k.shape[2]
    z = jnp.einsum('bhqd,bhkd->bhqk', q_blk, k).astype(jnp.float32) * (SB_HEAD_DIM ** -0.5)
    mask = jnp.arange(L)[None, :] < q_pos[:, None]
    log_1m = jnp.where(mask, jax.nn.log_sigmoid(-z), 0.0)
    between = lax.cumsum(log_1m, axis=3, reverse=True) - log_1m
    a = jnp.where(mask, jnp.exp(jax.nn.log_sigmoid(z) + between), 0.0)
    return jnp.einsum('bhqk,bhkd->bhqd', a, v.astype(jnp.float32)).astype(v.dtype)


def stick_breaking_attention(q, k, v):
    b, h, L, d = q.shape
    n_real = L - N_META
    o_meta = sb_query_block(q[:, :, :N_META], jnp.arange(N_META), k, v)
    starts = N_META + jnp.arange(n_real // SB_BLOCK) * SB_BLOCK

    def body(start):
        qb = lax.dynamic_slice_in_dim(q, start, SB_BLOCK, axis=2)
        return sb_query_block(qb, start + jnp.arange(SB_BLOCK), k, v)

    o_real = lax.map(body, starts)
    o_real = jnp.moveaxis(o_real, 0, 2).reshape(b, h, n_real, d)
    return jnp.concatenate([o_meta, o_real], axis=2)


def gla_chunk(state, xs):
    q, k, v, lg = xs
    C = q.shape[2]
    bcum = jnp.cumsum(lg, axis=2)
    o_inter = jnp.einsum('bhtd,bhde->bhte', q * jnp.exp(bcum), state)
    causal = (jnp.arange(C)[:, None] >= jnp.arange(C)[None, :])[:, :, None]
    diff = bcum[:, :, :, None, :] - bcum[:, :, None, :, :]
    decay = jnp.where(causal, jnp.exp(jnp.where(causal, diff, 0.0)), 0.0)
    attn = jnp.einsum('bhtd,bhsd,bhtsd->bhts', q, k, decay)
    o = o_inter + jnp.einsum('bhts,bhse->bhte', attn, v)
    b_last = bcum[:, :, -1:, :]
    new_state = jnp.exp(b_last[:, :, 0, :])[..., None] * state + \
        jnp.einsum('bhsd,bhse->bhde', k * jnp.exp(b_last - bcum), v)
    return new_state, o


def gated_linear_attention(q, k, v, lg):
    out_dtype = v.dtype
    q, k, v, lg = (t.astype(jnp.float32) for t in (q, k, v, lg))
    b, h, L, dk = q.shape
    dv = v.shape[-1]
    n_real = L - N_META
    n_chunks = n_real // GLA_CHUNK
    state0 = jnp.zeros((b, h, dk, dv), jnp.float32)
    state, o_meta = gla_chunk(state0, (q[:, :, :N_META], k[:, :, :N_META], v[:, :, :N_META], lg[:, :, :N_META]))

    def to_chunks(t):
        return jnp.moveaxis(t[:, :, N_META:].reshape(b, h, n_chunks, GLA_CHUNK, t.shape[-1]), 2, 0)

    _, o_real = lax.scan(gla_chunk, state, (to_chunks(q), to_chunks(k), to_chunks(v), to_chunks(lg)))
    o_real = jnp.moveaxis(o_real, 0, 2).reshape(b, h, n_real, dv)
    return jnp.concatenate([o_meta, o_real], axis=2).astype(out_dtype)


def hybrid_mixer(hn, w_in, w_gate_up, b_gate_up, sb_norm_g, gla_norm_g, w_out):
    proj = hn @ w_in
    offsets = list(np.cumsum(IN_SPLITS)[:-1])
    sbq, sbk, sbv, gq, gk, gv, gr, glr = jnp.split(proj, offsets, axis=-1)
    sb_o = stick_breaking_attention(split_heads(sbq, SB_HEADS), split_heads(sbk, SB_HEADS),
                                    split_heads(sbv, SB_HEADS))
    sb_o = merge_heads(rmsnorm(sb_o, sb_norm_g.reshape(SB_HEADS, 1, SB_HEAD_DIM)))
    lg = jax.nn.log_sigmoid((glr @ w_gate_up + b_gate_up).astype(jnp.float32)) / GLA_TAU
    gla_o = gated_linear_attention(split_heads(gq, GLA_HEADS) * (GLA_DK ** -0.5),
                                   split_heads(gk, GLA_HEADS), split_heads(gv, GLA_HEADS),
                                   split_heads(lg, GLA_HEADS))
    gla_o = merge_heads(rmsnorm(gla_o, gla_norm_g.reshape(GLA_HEADS, 1, GLA_DV))) * jax.nn.silu(gr)
    return jnp.concatenate([sb_o, gla_o], axis=-1) @ w_out


def peer_ffn(hn, w_peer_q, sub_keys1, sub_keys2, expert_u, expert_v):
    b, L, d = hn.shape
    t = hn.reshape(b * L, d)
    T = t.shape[0]
    q = (t @ w_peer_q).reshape(T, PEER_HEADS, 2, PEER_HALF)
    s1 = jnp.einsum('thd,hkd->thk', q[:, :, 0], sub_keys1).astype(jnp.float32)
    s2 = jnp.einsum('thd,hkd->thk', q[:, :, 1], sub_keys2).astype(jnp.float32)
    v1, i1 = lax.top_k(s1, PEER_TOPK)
    v2, i2 = lax.top_k(s2, PEER_TOPK)
    cand = (v1[..., :, None] + v2[..., None, :]).reshape(T, PEER_HEADS, PEER_TOPK * PEER_TOPK)
    cidx = (i1[..., :, None] * PEER_NKEYS + i2[..., None, :]).reshape(T, PEER_HEADS, PEER_TOPK * PEER_TOPK)
    sc, pos = lax.top_k(cand, PEER_TOPK)
    eidx = jnp.take_along_axis(cidx, pos, axis=-1).reshape(T, PEER_HEADS * PEER_TOPK)
    gates = jax.nn.softmax(sc, axis=-1).reshape(T, PEER_HEADS * PEER_TOPK).astype(hn.dtype)
    n_blocks = -(-T // PEER_BLOCK)
    pad = n_blocks * PEER_BLOCK - T
    tb = jnp.pad(t, ((0, pad), (0, 0))).reshape(n_blocks, PEER_BLOCK, d)
    eb = jnp.pad(eidx, ((0, pad), (0, 0))).reshape(n_blocks, PEER_BLOCK, -1)
    gb = jnp.pad(gates, ((0, pad), (0, 0))).reshape(n_blocks, PEER_BLOCK, -1)

    def block(args):
        xt, e, g = args
        act = jax.nn.gelu(jnp.einsum('nkd,nd->nk', expert_u[e], xt), approximate=False)
        return jnp.einsum('nk,nkd->nd', g * act, expert_v[e])

    y = lax.map(block, (tb, eb, gb)).reshape(n_blocks * PEER_BLOCK, d)[:T]
    return y.reshape(b, L, d)


def setup_inputs(seed: int = 0) -> dict:
    key = jax.random.key(seed)
    ks = jax.random.split(key, 20)
    n = jax.random.normal
    f = jnp.float32
    return {
        "x": n(ks[0], (BATCH, SEQ, D_MODEL), f),
        "meta_tokens": n(ks[1], (N_META, D_MODEL), f),
        "norm1_g": 1.0 + 0.02 * n(ks[2], (DEPTH, D_MODEL), f),
        "w_in": n(ks[3], (DEPTH, D_MODEL, IN_COLS), f) * D_MODEL ** -0.5,
        "w_gate_up": n(ks[4], (DEPTH, GLA_GATE_RANK, GLA_KW), f) * GLA_GATE_RANK ** -0.5,
        "b_gate_up": 0.1 * n(ks[5], (DEPTH, GLA_KW), f),
        "sb_norm_g": 1.0 + 0.02 * n(ks[6], (DEPTH, SB_WIDTH), f),
        "gla_norm_g": 1.0 + 0.02 * n(ks[7], (DEPTH, GLA_WIDTH), f),
        "w_out": n(ks[8], (DEPTH, MIX_WIDTH, D_MODEL), f) * MIX_WIDTH ** -0.5,
        "norm2_g": 1.0 + 0.02 * n(ks[9], (DEPTH, D_MODEL), f),
        "w_peer_q": n(ks[10], (DEPTH, D_MODEL, PEER_HEADS * PEER_QDIM), f) * D_MODEL ** -0.5,
        "sub_keys1": n(ks[11], (DEPTH, PEER_HEADS, PEER_NKEYS, PEER_HALF), f) * PEER_HALF ** -0.5,
        "sub_keys2": n(ks[12], (DEPTH, PEER_HEADS, PEER_NKEYS, PEER_HALF), f) * PEER_HALF ** -0.5,
        "expert_u": n(ks[13], (DEPTH, N_EXPERTS, D_MODEL), f) * D_MODEL ** -0.5,
        "expert_v": n(ks[14], (DEPTH, N_EXPERTS, D_MODEL), f) * PEER_HEADS ** -0.5,
        "norm_f_g": 1.0 + 0.02 * n(ks[15], (D_MODEL,), f),
    }


def reference(x, meta_tokens, norm1_g, w_in, w_gate_up, b_gate_up, sb_norm_g, gla_norm_g, w_out,
              norm2_g, w_peer_q, sub_keys1, sub_keys2, expert_u, expert_v, norm_f_g):
    b = x.shape[0]
    meta = jnp.broadcast_to(meta_tokens.astype(x.dtype)[None], (b, N_META, x.shape[-1]))
    h = jnp.concatenate([meta, x], axis=1)
    for l in range(DEPTH):
        h = h + hybrid_mixer(rmsnorm(h, norm1_g[l]), w_in[l], w_gate_up[l], b_gate_up[l],
                             sb_norm_g[l], gla_norm_g[l], w_out[l])
        h = h + peer_ffn(rmsnorm(h, norm2_g[l]), w_peer_q[l], sub_keys1[l], sub_keys2[l],
                         expert_u[l], expert_v[l])
    return rmsnorm(h, norm_f_g)[:, N_META:]
```

```python
from contextlib import ExitStack
import numpy as np
import concourse.bass as bass
import concourse.mybir as mybir
from concourse.bass_utils import run_bass_kernel_spmd

F32 = mybir.dt.float32
BF16 = mybir.dt.bfloat16
U32 = mybir.dt.uint32
ALU = mybir.AluOpType
AF = mybir.ActivationFunctionType
AX = mybir.AxisListType

D = 1024
NMETA = 16
EPS = 1e-6
INC = 3088
NEG = -1e30


class FW:
    def __init__(self, nc):
        self.nc = nc
        self.eng = {"pe": nc.tensor, "act": nc.scalar, "dve": nc.vector, "pool": nc.gpsimd, "sp": nc.sync}
        self.sem = {}
        self.cnt = {}
        for e in ("pe", "act", "dve", "pool"):
            self.sem[e] = nc.alloc_semaphore(f"s_{e}")
            self.cnt[e] = 0
        self.seen = {}
        self.lastw = {}
        self.readers = {}
        self.dsem = {}
        self.pending = {e: False for e in ("pe", "act", "dve", "pool")}

    def _wait(self, engname, tok):
        if tok is None:
            return
        semname, sem, val = tok
        if semname == engname and val > self.cnt[engname]:
            return
        k = (engname, semname)
        if self.seen.get(k, 0) >= val:
            return
        self.seen[k] = val
        self.eng[engname].wait_ge(sem, val)

    def deps(self, engname, reads, writes):
        for b in reads:
            self._wait(engname, self.lastw.get(b))
        for b in writes:
            self._wait(engname, self.lastw.get(b))
            for t in self.readers.get(b, ()):
                self._wait(engname, t)

    def commit(self, tok, reads, writes):
        for b in reads:
            self.readers.setdefault(b, []).append(tok)
        for b in writes:
            self.lastw[b] = tok
            self.readers[b] = []

    @staticmethod
    def _norm(reads, writes):
        isps = lambda b: b.startswith("ps") and len(b) > 2 and b[2].isdigit()
        r2 = [b for b in reads if not isps(b)]
        w2 = [b[:3] if isps(b) else b for b in writes] + [b[:3] for b in reads if isps(b)]
        return r2, w2

    def op(self, engname, fn, reads=(), writes=(), signal=True):
        reads, writes = self._norm(reads, writes)
        self.deps(engname, reads, writes)
        ins = fn()
        if signal:
            self.cnt[engname] += 1
            ins.then_inc(self.sem[engname], 1)
            tok = (engname, self.sem[engname], self.cnt[engname])
            self.pending[engname] = False
        else:
            tok = (engname, self.sem[engname], self.cnt[engname] + 1)
            self.pending[engname] = True
        self.commit(tok, reads, writes)
        return tok

    def dma(self, slot, out, in_, reads=(), writes=(), q="sp", **kw):
        if slot not in self.dsem:
            self.dsem[slot] = [self.nc.alloc_semaphore(f"d_{slot}"), 0]
        reads, writes = self._norm(reads, writes)
        self.deps(q, reads, writes)
        ins = self.eng[q].dma_start(out=out, in_=in_, **kw)
        d = self.dsem[slot]
        d[1] += 16
        ins.then_inc(d[0], 16)
        tok = ("d_" + slot, d[0], d[1])
        self.commit(tok, reads, writes)
        return tok

    def barrier(self):
        assert not any(self.pending.values())
        toks = [(e, self.sem[e], self.cnt[e]) for e in ("pe", "act", "dve", "pool") if self.cnt[e] > 0]
        toks += [("d_" + s, d[0], d[1]) for s, d in self.dsem.items()]
        for e in ("pe", "act", "dve", "pool", "sp"):
            for t in toks:
                self._wait(e, t)
        self.lastw.clear()
        self.readers.clear()


STOP = 99
SKIP_SETUP = False
NCH_DBG = 0
DBG = 'uvd'
P1STOP = 99


def build(NR):
    NG = NR // 512
    NT = NR // 128
    L = NR + NMETA
    nc = bass.Bass("TRN2", target_bir_lowering=False)
    dt = lambda n, s, k="ExternalInput", d=F32: nc.dram_tensor(n, s, d, kind=k).ap()
    x = dt("x", [NR, D])
    meta = dt("meta_tokens", [NMETA, D])
    norm1_g = dt("norm1_g", [1, D])
    w_in = dt("w_in", [D, INC])
    w_gate_up = dt("w_gate_up", [16, 256])
    b_gate_up = dt("b_gate_up", [1, 256])
    sb_norm_g = dt("sb_norm_g", [1, 512])
    gla_norm_g = dt("gla_norm_g", [1, 512])
    w_out = dt("w_out", [D, D])
    norm2_g = dt("norm2_g", [1, D])
    w_peer_q = dt("w_peer_q", [D, D])
    sub_keys1 = dt("sub_keys1", [8, 128, 64])
    sub_keys2 = dt("sub_keys2", [8, 128, 64])
    expert_u = dt("expert_u", [16384, D])
    expert_v = dt("expert_v", [16384, D])
    norm_f_g = dt("norm_f_g", [1, D])
    out = dt("out", [NR, D], "ExternalOutput")
    QT = nc.dram_tensor("QT", [512, NR], BF16).ap()
    KT = nc.dram_tensor("KT", [512, L], BF16).ap()
    VT = nc.dram_tensor("VT", [L, 512], BF16).ap()
    MIXT = nc.dram_tensor("MIXT", [D, NR], BF16).ap()
    UT = nc.dram_tensor("UT", [128, 128, 8, 128], BF16).ap()
    VB = nc.dram_tensor("VB", [16384, D], BF16).ap()

    fw = FW(nc)
    V, A, P, G, SP = nc.vector, nc.scalar, nc.tensor, nc.gpsimd, nc.sync
    PS = [nc.alloc_psum_tensor(f"ps{i}", [128, 512], F32) for i in range(8)]

    glob = ExitStack()

    def sb(stack, name, shape, dtype):
        return stack.enter_context(nc.sbuf_tensor(name, shape, dtype))

    ident_f = sb(glob, "ident_f", [128, 128], F32)
    ident_b = sb(glob, "ident_b", [128, 128], BF16)
    ones_b = sb(glob, "ones_b", [128, 128], BF16)
    iot = sb(glob, "iot", [128, 128], F32)
    iotc = sb(glob, "iotc", [128, 128], F32)
    fw.op("pool", lambda: G.iota(iot[:, :], pattern=[[1, 128]], base=0, channel_multiplier=-1,
                                 allow_small_or_imprecise_dtypes=True), writes=["iot"])
    fw.op("pool", lambda: G.iota(iotc[:, :], pattern=[[1, 128]], base=0, channel_multiplier=0,
                                 allow_small_or_imprecise_dtypes=True), writes=["iotc"])
    fw.op("dve", lambda: V.tensor_single_scalar(out=ident_f[:, :], in_=iot[:, :], scalar=0.0, op=ALU.is_equal),
          reads=["iot"], writes=["ident_f"])
    fw.op("dve", lambda: V.tensor_copy(out=ident_b[:, :], in_=ident_f[:, :]), reads=["ident_f"], writes=["ident_b"])
    fw.op("dve", lambda: V.memset(ones_b[:, :], 1.0), writes=["ones_b"])

    def rstd_from_ss(ss, rs, n, key_ss, key_rs, npart=128):
        fw.op("act", lambda: A.activation(out=rs[:npart, :], in_=ss[:npart, :], func=AF.Ln, scale=1.0 / n, bias=epsc[:npart, :]),
              reads=[key_ss], writes=[key_rs])
        fw.op("act", lambda: A.activation(out=rs[:npart, :], in_=rs[:npart, :], func=AF.Exp, scale=-0.5),
              reads=[key_rs], writes=[key_rs])

    epsc = sb(glob, "epsc", [128, 1], F32)
    fw.op("dve", lambda: V.memset(epsc[:, :], EPS), writes=["epsc"])
    fw.barrier()

    if STOP < 1:
        return nc
    with ExitStack() as st:
        win = sb(st, "win", [128, 8, INC], BF16)
        wstg = [sb(st, f"wstg{i}", [128, INC], F32) for i in range(2)]
        for k in range(8):
            s = k % 2
            fw.dma(f"wstg{s}", wstg[s][:, :], w_in[k * 128:(k + 1) * 128, :], writes=[f"wstg{s}"])
            e = ("act", "dve", "pool")[k % 3]
            if e == "act":
                fw.op("act", lambda: A.copy(out=win[:, k, :], in_=wstg[s][:, :]), reads=[f"wstg{s}"], writes=["win"])
            elif e == "dve":
                fw.op("dve", lambda: V.tensor_copy(out=win[:, k, :], in_=wstg[s][:, :]), reads=[f"wstg{s}"], writes=["win"])
            else:
                fw.op("pool", lambda: G.tensor_copy(out=win[:, k, :], in_=wstg[s][:, :]), reads=[f"wstg{s}"], writes=["win"])
        g1b = sb(st, "g1b", [128, D], F32)
        fw.dma("c1", g1b[:, :], norm1_g[0:1, :].partition_broadcast(128).rearrange("p o n -> p (o n)"), writes=["g1b"])
        wgu_f = sb(st, "wgu_f", [16, 256], F32)
        wgu = sb(st, "wgu", [16, 256], BF16)
        bgu_f = sb(st, "bgu_f", [1, 256], F32)
        bgu = sb(st, "bgu", [1, 256], BF16)
        fw.dma("c2", wgu_f[:, :], w_gate_up[:, :], writes=["wgu_f"])
        fw.dma("c3", bgu_f[:, :], b_gate_up[:, :], writes=["bgu_f"])
        fw.op("dve", lambda: V.tensor_copy(out=wgu[:, :], in_=wgu_f[:, :]), reads=["wgu_f"], writes=["wgu"])
        fw.op("dve", lambda: V.tensor_copy(out=bgu[:, :], in_=bgu_f[:, :]), reads=["bgu_f"], writes=["bgu"])
        ggla = sb(st, "ggla", [128, 4], F32)
        fw.dma("c4", ggla[:, :], gla_norm_g.rearrange("o (h e) -> e (o h)", e=128), writes=["ggla"],
               allow_slow_non_contiguous=True)
        triI = sb(st, "triI", [128, 128], F32)
        strA = sb(st, "strA", [128, 128], F32)
        m01 = sb(st, "m01", [128, 128], F32)
        fw.op("dve", lambda: V.tensor_scalar(out=triI[:, :], in0=iot[:, :], scalar1=0.0, scalar2=-1.0 / 16, op0=ALU.is_ge, op1=ALU.mult),
              reads=["iot"], writes=["triI"])
        fw.op("dve", lambda: V.tensor_scalar(out=strA[:, :], in0=iot[:, :], scalar1=0.0, scalar2=-1.0 / 16, op0=ALU.is_lt, op1=ALU.mult),
              reads=["iot"], writes=["strA"])
        fw.op("dve", lambda: V.tensor_single_scalar(out=m01[:, :], in_=iot[:, :], scalar=0.0, op=ALU.is_ge),
              reads=["iot"], writes=["m01"])

        xs = [sb(st, f"xs{i}", [128, D], F32) for i in range(2)]
        junk = sb(st, "junk", [128, D], BF16)
        ss = sb(st, "ss", [128, 1], F32)
        rs = sb(st, "rs", [128, 1], F32)
        hn = sb(st, "hn", [128, D], BF16)
        hnT = sb(st, "hnT", [128, 8, 512], BF16)
        qk_o = [sb(st, f"qko{i}", [128, 512], BF16) for i in range(2)]
        gqT = sb(st, "gqT", [64, 4, 512], F32)
        gkT = sb(st, "gkT", [64, 4, 512], F32)
        grS = sb(st, "grS", [128, 4, 512], F32)
        glrT = sb(st, "glrT", [16, 512], BF16)
        ones1 = sb(st, "ones1", [1, 128], BF16)
        fw.op("dve", lambda: V.memset(ones1[:, :], 1.0), writes=["ones1"])
        vtok = [sb(st, f"vtok{i}", [128, 512], BF16) for i in range(2)]
        gv = sb(st, "gv", [128, 512], BF16)
        gkt = sb(st, "gkt", [128, 256], F32)
        spl = sb(st, "spl", [128, 256], F32)
        eT = sb(st, "eT", [64, 4, 128], F32)
        enT = sb(st, "enT", [64, 4, 128], F32)
        qtl = sb(st, "qtl", [64, 4, 128], BF16)
        ktl = sb(st, "ktl", [64, 4, 128], BF16)
        ebd = sb(st, "ebd", [128, 256], F32)
        khat = sb(st, "khat", [128, 256], BF16)
        attn = sb(st, "attn", [128, 4, 128], BF16)
        state = sb(st, "state", [64, 4, 128], F32)
        stb = sb(st, "stb", [64, 4, 128], BF16)
        osq = sb(st, "osq", [128, 512], BF16)
        rso = sb(st, "rso", [128, 512], F32)
        o1 = sb(st, "o1", [128, 512], F32)
        mixg = [sb(st, f"mixg{i}", [128, 4, 128], BF16) for i in range(2)]
        fw.op("dve", lambda: V.memset(state[:, :, :], 0.0), writes=["state"])
        fw.op("dve", lambda: V.memset(stb[:, :, :], 0.0), writes=["stb"])

        if P1STOP == 0:
            fw.barrier()
            return nc
        ust = [sb(st, f"ust{i}", [128, D], F32) for i in range(2)]
        vst = [sb(st, f"vst{i}", [128, D], F32) for i in range(2)]
        utb = [sb(st, f"utb{i}", [128, 8, 128], BF16) for i in range(2)]
        vbb = [sb(st, f"vbb{i}", [128, D], BF16) for i in range(2)]

        def setup_gen():
            def load(c):
                s_ = c % 2
                fw.dma(f"ust{s_}", ust[s_][:, :], expert_u[c * 128:(c + 1) * 128, :], writes=[f"ust{s_}"])
                fw.dma(f"vst{s_}", vst[s_][:, :], expert_v[c * 128:(c + 1) * 128, :], writes=[f"vst{s_}"])

            def work(c):
                s_ = c % 2
                for hlf in range(2):
                    pt = PS[hlf]
                    for k4 in range(4):
                        k = hlf * 4 + k4
                        fw.op("pe", lambda: P.transpose(out=pt[:, k4 * 128:(k4 + 1) * 128], in_=ust[s_][:, k * 128:(k + 1) * 128],
                                                        identity=ident_f[:, :]),
                              reads=[f"ust{s_}"], writes=[f"ps{hlf}"], signal=(k4 == 3))
                    if hlf == 0:
                        fw.op("act", lambda: A.copy(out=utb[s_][:, 0:4, :].rearrange("p a b -> p (a b)"), in_=pt[:, :]),
                              reads=[f"ps{hlf}"], writes=[f"utb{s_}_0"])
                    else:
                        fw.op("dve", lambda: V.tensor_copy(out=utb[s_][:, 4:8, :].rearrange("p a b -> p (a b)"), in_=pt[:, :]),
                              reads=[f"ps{hlf}"], writes=[f"utb{s_}_1"])
                fw.op("pool", lambda: G.tensor_copy(out=vbb[s_][:, :], in_=vst[s_][:, :]), reads=[f"vst{s_}"], writes=[f"vbb{s_}"])

            def store(c):
                s_ = c % 2
                fw.dma(f"utb{s_}", UT[c], utb[s_][:, :, :], reads=[f"utb{s_}_0", f"utb{s_}_1"], writes=["UT"])
                fw.dma(f"vbb{s_}", VB[c * 128:(c + 1) * 128, :], vbb[s_][:, :], reads=[f"vbb{s_}"], writes=["VB"])

            nch = NCH_DBG if SKIP_SETUP else 128
            if nch == 0:
                return
            load(0)
            yield
            for c in range(nch):
                if c + 1 < nch:
                    load(c + 1)
                work(c)
                if c >= 1:
                    store(c - 1)
                yield
            store(nch - 1)
            yield

        sg = setup_gen()
        tile_ctr = [0]
        for g in range(-1, NG):
            n = NMETA if g < 0 else 512
            ntile = 1 if g < 0 else 4
            tn = NMETA if g < 0 else 128
            for tl in range(ntile):
                s = tile_ctr[0] % 2
                tile_ctr[0] += 1
                src = meta[:, :] if g < 0 else x[g * 512 + tl * 128: g * 512 + (tl + 1) * 128, :]
                fw.dma(f"xs{s}", xs[s][:tn, :], src, writes=[f"xs{s}"])
                fw.op("act", lambda: A.activation(out=junk[:tn, :], in_=xs[s][:tn, :], func=AF.Square, accum_out=ss[:tn, :]),
                      reads=[f"xs{s}"], writes=["junk", "ss"])
                rstd_from_ss(ss, rs, D, "ss", "rs", tn)
                fw.op("dve", lambda: V.scalar_tensor_tensor(out=hn[:tn, :], in0=xs[s][:tn, :], scalar=rs[:tn, :], in1=g1b[:tn, :],
                                                            op0=ALU.mult, op1=ALU.mult),
                      reads=[f"xs{s}", "rs", "g1b"], writes=["hn"])
                ptr = PS[0][:, :].bitcast(BF16)
                for k in range(8):
                    fw.op("pe", lambda: P.transpose(out=ptr[:, k * 128:k * 128 + tn], in_=hn[:tn, k * 128:(k + 1) * 128],
                                                    identity=ident_b[:tn, :tn]),
                          reads=["hn"], writes=["ps0"], signal=(k == 7))
                fw.op("act", lambda: A.copy(out=hnT[:, :, tl * 128:tl * 128 + tn],
                                            in_=ptr.rearrange("p (k t) -> p k t", k=8)[:, :, :tn]),
                      reads=["ps0"], writes=["hnT"])
            if P1STOP == 1:
                fw.barrier()
                return nc
            def fproj(col0, m, evac):
                for k in range(8):
                    fw.op("pe", lambda: P.matmul(out=PS[1][:m, :n], lhsT=win[:, k, col0:col0 + m], rhs=hnT[:, k, :n],
                                                 start=(k == 0), stop=(k == 7)),
                          reads=["win", "hnT"], writes=["ps1"], signal=(k == 7))
                evac(PS[1][:m, :n])
            qctr = [0]
            if g >= 0:
                for c in range(4):
                    def ev(ps, c=c):
                        s = qctr[0] % 2
                        qctr[0] += 1
                        fw.op("act", lambda: A.copy(out=qk_o[s][:, :n], in_=ps), reads=["ps1"], writes=[f"qko{s}"])
                        fw.dma(f"qko{s}", QT[c * 128:(c + 1) * 128, g * 512:(g + 1) * 512], qk_o[s][:, :n],
                               reads=[f"qko{s}"], writes=["QT"])
                    fproj(c * 128, 128, ev)
            kcol0 = 0 if g < 0 else NMETA + g * 512
            for c in range(4):
                def ev(ps, c=c):
                    s = qctr[0] % 2
                    qctr[0] += 1
                    fw.op("dve", lambda: V.tensor_copy(out=qk_o[s][:, :n], in_=ps), reads=["ps1"], writes=[f"qko{s}"])
                    fw.dma(f"qko{s}", KT[c * 128:(c + 1) * 128, kcol0:kcol0 + n], qk_o[s][:, :n],
                           reads=[f"qko{s}"], writes=["KT"])
                fproj(512 + c * 128, 128, ev)
            for c in range(4):
                fproj(1536 + c * 64, 64, lambda ps, c=c: fw.op(
                    "act", lambda: A.copy(out=gqT[:, c, :n], in_=ps), reads=["ps1"], writes=["gqT"]))
                fproj(1792 + c * 64, 64, lambda ps, c=c: fw.op(
                    "dve", lambda: V.tensor_copy(out=gkT[:, c, :n], in_=ps), reads=["ps1"], writes=["gkT"]))
            if g >= 0:
                for c in range(4):
                    fproj(2560 + c * 128, 128, lambda ps, c=c: fw.op(
                        "act", lambda: A.activation(out=grS[:, c, :n], in_=ps, func=AF.Silu), reads=["ps1"], writes=["grS"]))
            fproj(3072, 16, lambda ps: fw.op("dve", lambda: V.tensor_copy(out=glrT[:, :n], in_=ps), reads=["ps1"], writes=["glrT"]))

            if P1STOP == 2:
                fw.barrier()
                return nc
            for tl in range(ntile):
                tsl = slice(tl * 128, tl * 128 + tn)
                tok0 = (0 if g < 0 else NMETA + g * 512 + tl * 128)
                def tproj(col0, w, evac):
                    for k in range(8):
                        fw.op("pe", lambda: P.matmul(out=PS[2][:tn, :w], lhsT=hnT[:, k, tsl], rhs=win[:, k, col0:col0 + w],
                                                     start=(k == 0), stop=(k == 7)),
                              reads=["win", "hnT"], writes=["ps2"], signal=(k == 7))
                    evac(PS[2][:tn, :w])
                vs = tile_ctr[0] % 2
                tile_ctr[0] += 1
                def ev_v(ps):
                    fw.op("act", lambda: A.copy(out=vtok[vs][:tn, :], in_=ps), reads=["ps2"], writes=[f"vtok{vs}"])
                    fw.dma(f"vtok{vs}", VT[tok0:tok0 + tn, :], vtok[vs][:tn, :], reads=[f"vtok{vs}"], writes=["VT"])
                tproj(1024, 512, ev_v)
                tproj(2048, 512, lambda ps: fw.op("dve", lambda: V.tensor_copy(out=gv[:tn, :], in_=ps), reads=["ps2"], writes=["gv"]))
                tproj(1792, 256, lambda ps: fw.op("act", lambda: A.copy(out=gkt[:tn, :], in_=ps), reads=["ps2"], writes=["gkt"]))
                if P1STOP == 3:
                    fw.barrier()
                    return nc
                fw.op("pe", lambda: P.matmul(out=PS[3][:tn, 0:256], lhsT=glrT[:, tsl], rhs=wgu[:, :], start=True, stop=False),
                      reads=["glrT", "wgu"], writes=["ps3a"], signal=False)
                fw.op("pe", lambda: P.matmul(out=PS[3][:tn, 0:256], lhsT=ones1[:, :tn], rhs=bgu[:, :], start=False, stop=True),
                      reads=["ones1", "bgu"], writes=["ps3a"])
                fw.op("act", lambda: A.activation(out=spl[:tn, :], in_=PS[3][:tn, 0:256], func=AF.Exp, scale=-1.0),
                      reads=["ps3a"], writes=["spl"])
                fw.op("act", lambda: A.activation(out=spl[:tn, :], in_=spl[:tn, :], func=AF.Ln, bias=1.0),
                      reads=["spl"], writes=["spl"])
                for c in range(4):
                    fw.op("pe", lambda: P.matmul(out=PS[4][:64, c * 128:c * 128 + tn], lhsT=spl[:tn, c * 64:(c + 1) * 64],
                                                 rhs=triI[:tn, :tn], start=True, stop=True),
                          reads=["spl", "triI"], writes=["ps4"], signal=(c == 3))
                fw.op("pe", lambda: P.matmul(out=PS[3][:tn, 256:512], lhsT=strA[:tn, :tn], rhs=spl[:tn, :], start=True, stop=True),
                      reads=["spl", "strA"], writes=["ps3b"])
                ps4v = PS[4][:64, :].rearrange("p (c t) -> p c t", c=4)[:, :, :tn]
                fw.op("act", lambda: A.activation(out=eT[:, :, :tn], in_=ps4v, func=AF.Exp), reads=["ps4"], writes=["eT"])
                fw.op("act", lambda: A.activation(out=enT[:, :, :tn], in_=ps4v, func=AF.Exp, scale=-1.0), reads=["ps4"], writes=["enT"])
                fw.op("act", lambda: A.activation(out=ebd[:tn, :], in_=PS[3][:tn, 256:512], func=AF.Exp), reads=["ps3b"], writes=["ebd"])
                if P1STOP == 4:
                    fw.barrier()
                    return nc
                fw.op("dve", lambda: V.scalar_tensor_tensor(out=qtl[:, :, :tn], in0=gqT[:, :, tsl], scalar=0.125, in1=eT[:, :, :tn],
                                                            op0=ALU.mult, op1=ALU.mult),
                      reads=["gqT", "eT"], writes=["qtl"])
                fw.op("pool", lambda: G.tensor_tensor(out=ktl[:, :, :tn], in0=gkT[:, :, tsl], in1=enT[:, :, :tn], op=ALU.mult),
                      reads=["gkT", "enT"], writes=["ktl"])
                fw.op("dve", lambda: V.tensor_tensor(out=khat[:tn, :], in0=gkt[:tn, :], in1=ebd[:tn, :], op=ALU.mult),
                      reads=["gkt", "ebd"], writes=["khat"])
                if g >= 0:
                    for h in range(4):
                        fw.op("pe", lambda: P.matmul(out=PS[5][:, h * 128:(h + 1) * 128], lhsT=ktl[:, h, :],
                                                     rhs=qtl[:, h, :], start=True, stop=True),
                              reads=["ktl", "qtl"], writes=["ps5"], signal=(h == 3))
                    fw.op("dve", lambda: V.tensor_tensor(out=attn[:, :, :], in0=PS[5][:, :].rearrange("p (h t) -> p h t", h=4),
                                                         in1=m01[:, :].unsqueeze(1).to_broadcast([128, 4, 128]), op=ALU.mult),
                          reads=["ps5", "m01"], writes=["attn"])
                    for h in range(4):
                        fw.op("pe", lambda: P.matmul(out=PS[6][:, h * 128:(h + 1) * 128], lhsT=gv[:, h * 128:(h + 1) * 128],
                                                     rhs=attn[:, h, :], start=True, stop=False),
                              reads=["gv", "attn"], writes=["ps6"], signal=False)
                        fw.op("pe", lambda: P.matmul(out=PS[6][:, h * 128:(h + 1) * 128], lhsT=stb[:, h, :],
                                                     rhs=qtl[:, h, :], start=False, stop=True),
                              reads=["stb", "qtl"], writes=["ps6"], signal=(h == 3))
                if P1STOP == 5:
                    fw.barrier()
                    return nc
                for h in range(4):
                    fw.op("pe", lambda: P.matmul(out=PS[7][:64, h * 128:(h + 1) * 128], lhsT=khat[:tn, h * 64:(h + 1) * 64],
                                                 rhs=gv[:tn, h * 128:(h + 1) * 128], start=True, stop=True),
                          reads=["khat", "gv"], writes=["ps7"], signal=(h == 3))
                for h in range(4):
                    fw.op("dve", lambda: V.scalar_tensor_tensor(
                        out=state[:, h, :], in0=state[:, h, :], scalar=eT[:, h, tn - 1:tn],
                        in1=PS[7][:64, h * 128:(h + 1) * 128],
                        op0=ALU.mult, op1=ALU.add), reads=["state", "eT", "ps7"], writes=["state"])
                fw.op("act", lambda: A.copy(out=stb[:, :, :], in_=state[:, :, :]), reads=["state"], writes=["stb"])
                if P1STOP == 6:
                    fw.barrier()
                    return nc
                if P1STOP == 7 and g >= 0:
                    fw.barrier()
                    return nc
                if g >= 0:
                    fw.op("act", lambda: A.activation(out=osq[:, :], in_=PS[6][:, :], func=AF.Square), reads=["ps6"], writes=["osq"])
                    fw.op("pe", lambda: P.matmul(out=PS[5][:, :], lhsT=ones_b[:, :], rhs=osq[:, :], start=True, stop=True),
                          reads=["osq", "ones_b"], writes=["ps5"])
                    fw.op("act", lambda: A.activation(out=rso[:, :], in_=PS[5][:, :], func=AF.Ln, scale=1.0 / 128, bias=epsc[:, :]),
                          reads=["ps5"], writes=["rso"])
                    fw.op("act", lambda: A.activation(out=rso[:, :], in_=rso[:, :], func=AF.Exp, scale=-0.5),
                          reads=["rso"], writes=["rso"])
                    fw.op("dve", lambda: V.tensor_tensor(out=o1[:, :], in0=PS[6][:, :], in1=rso[:, :], op=ALU.mult),
                          reads=["ps6", "rso"], writes=["o1"])
                    ms = tile_ctr[0] % 2
                    for h in range(4):
                        fw.op("dve", lambda: V.scalar_tensor_tensor(out=mixg[ms][:, h, :], in0=o1[:, h * 128:(h + 1) * 128],
                                                                    scalar=ggla[:, h:h + 1], in1=grS[:, h, tsl],
                                                                    op0=ALU.mult, op1=ALU.mult),
                              reads=["o1", "ggla", "grS"], writes=[f"mixg{ms}"])
                    t0 = g * 512 + tl * 128
                    fw.dma(f"mixg{ms}", MIXT[512:1024, t0:t0 + 128].rearrange("(h e) t -> e h t", h=4), mixg[ms][:, :, :],
                           reads=[f"mixg{ms}"], writes=["MIXT"])
                next(sg, None)
                next(sg, None)
        for _ in sg:
            pass
        fw.barrier()

    if STOP < 2:
        return nc
    with ExitStack() as st:
        kts = [sb(st, f"kts{i}", [128, L], BF16) for i in range(2)]
        vts = [sb(st, f"vts{i}", [128, NT, 128], BF16) for i in range(2)]
        vmeta = [sb(st, f"vmeta{i}", [16, 128], BF16) for i in range(2)]
        qts = [sb(st, f"qts{i}", [128, 512], BF16) for i in range(2)]
        gsb = sb(st, "gsb", [128, 4], F32)
        fw.dma("c1", gsb[:, :], sb_norm_g.rearrange("o (c p) -> p (o c)", p=128), writes=["gsb"], allow_slow_non_contiguous=True)
        triK = sb(st, "triK", [128, 128], BF16)
        fw.op("dve", lambda: V.tensor_single_scalar(out=triK[:, :], in_=iot[:, :], scalar=0.0, op=ALU.is_le),
              reads=["iot"], writes=["triK"])
        ones2 = sb(st, "ones2", [128, 128], BF16)
        fw.op("dve", lambda: V.memset(ones2[:, :], 0.0), writes=["ones2"])
        fw.op("dve", lambda: V.memset(ones2[0:64, 0:64], 1.0), writes=["ones2"])
        fw.op("dve", lambda: V.memset(ones2[64:128, 64:128], 1.0), writes=["ones2"])
        dmask = sb(st, "dmask", [128, 4, 512], BF16)
        iot4 = sb(st, "iot4", [128, 512], F32)
        fw.op("pool", lambda: G.iota(iot4[:, :], pattern=[[1, 512]], base=0, channel_multiplier=-1,
                                     allow_small_or_imprecise_dtypes=True), writes=["iot4"])
        for a_ in range(4):
            fw.op("dve", lambda: V.tensor_single_scalar(out=dmask[:, a_, :], in_=iot4[:, :], scalar=float(128 * a_), op=ALU.is_gt),
                  reads=["iot4"], writes=["dmask"])
        NB = 4
        ee = [[sb(st, f"ee{u}_{i}", [128, 512], BF16) for i in range(NB)] for u in range(2)]
        spb = [[sb(st, f"spb{u}_{i}", [128, 512], BF16) for i in range(NB)] for u in range(2)]
        ww = [[sb(st, f"ww{u}_{i}", [128, 512], BF16) for i in range(NB)] for u in range(2)]
        aa = [[sb(st, f"aa{u}_{i}", [128, 512], BF16) for i in range(NB)] for u in range(2)]
        Sacc = [[sb(st, f"Sacc{u}_{i}", [128, 512], BF16) for i in range(2)] for u in range(2)]
        osq2 = sb(st, "osq2", [128, 512], BF16)
        rs2 = sb(st, "rs2", [128, 512], F32)
        mo = [sb(st, f"mo{i}", [128, 512], BF16) for i in range(2)]

        tctr = [0]
        gctr = [0]

        def load_kv(hp_):
            hs_ = hp_ % 2
            fw.dma(f"kts{hs_}", kts[hs_][:, :], KT[hp_ * 128:(hp_ + 1) * 128, :], writes=[f"kts{hs_}"])
            fw.dma(f"vts{hs_}", vts[hs_][:, :, :], VT[NMETA:L, hp_ * 128:(hp_ + 1) * 128].rearrange("(j p) d -> p j d", p=128),
                   writes=[f"vts{hs_}"])
            fw.dma(f"vmeta{hs_}", vmeta[hs_][:, :], VT[0:NMETA, hp_ * 128:(hp_ + 1) * 128], writes=[f"vmeta{hs_}"])

        def load_q(idx):
            hp_, g_ = divmod(idx, NG)
            gs_ = idx % 2
            fw.dma(f"qts{gs_}", qts[gs_][:, :], QT[hp_ * 128:(hp_ + 1) * 128, g_ * 512:(g_ + 1) * 512], writes=[f"qts{gs_}"])

        load_kv(0)
        load_q(0)
        for hp in range(4):
            hs = hp % 2
            for g in range(NG):
                gs = gctr[0] % 2
                gctr[0] += 1
                if g == 0 and hp + 1 < 4:
                    load_kv(hp + 1)
                if hp * NG + g + 1 < 4 * NG:
                    load_q(hp * NG + g + 1)
                blocks = [(4 * g + a_, a_) for a_ in (3, 2, 1, 0)] + [(j, None) for j in range(4 * g - 1, -1, -1)] + [(-1, None)]
                nb = len(blocks)

                def stage_z(i):
                    j, a_ = blocks[i]
                    t = tctr[0] + i
                    kn = 16 if j < 0 else 128
                    kc0 = 0 if j < 0 else NMETA + j * 128
                    s3 = t % NB
                    for u in range(2):
                        zb = 2 * u + t % 2
                        po = 64 * u
                        fw.op("pe", lambda: P.matmul(out=PS[zb][:kn, :], lhsT=kts[hs][po:po + 64, kc0:kc0 + kn],
                                                     rhs=qts[gs][po:po + 64, :], start=True, stop=True),
                              reads=[f"kts{hs}", f"qts{gs}"], writes=[f"ps{zb}"])
                    for u in range(2):
                        zb = 2 * u + t % 2
                        fw.op("act", lambda: A.activation(out=ee[u][s3][:kn, :], in_=PS[zb][:kn, :], func=AF.Exp, scale=0.125),
                              reads=[f"ps{zb}"], writes=[f"ee{u}_{s3}"])
                        fw.op("act", lambda: A.activation(out=spb[u][s3][:kn, :], in_=ee[u][s3][:kn, :], func=AF.Ln, bias=1.0),
                              reads=[f"ee{u}_{s3}"], writes=[f"spb{u}_{s3}"])
                        if a_ is not None:
                            fw.op("pool", lambda: G.tensor_tensor(out=spb[u][s3][:, :], in0=spb[u][s3][:, :], in1=dmask[:, a_, :],
                                                                  op=ALU.mult),
                                  reads=[f"spb{u}_{s3}", "dmask"], writes=[f"spb{u}_{s3}"])
                            fw.op("pool", lambda: G.tensor_tensor(out=ee[u][s3][:, :], in0=ee[u][s3][:, :], in1=dmask[:, a_, :],
                                                                  op=ALU.mult),
                                  reads=[f"ee{u}_{s3}", "dmask"], writes=[f"ee{u}_{s3}"])

                def stage_c(i):
                    j, a_ = blocks[i]
                    t = tctr[0] + i
                    s3 = t % NB
                    kn = 16 if j < 0 else 128
                    first = (i == 0)
                    for u in range(2):
                        cb = 4 + u
                        fw.op("pe", lambda: P.matmul(out=PS[cb][:kn, :], lhsT=triK[:kn, :kn], rhs=spb[u][s3][:kn, :],
                                                     start=True, stop=first),
                              reads=["triK", f"spb{u}_{s3}"], writes=[f"ps{cb}"], signal=first)
                        if not first:
                            fw.op("pe", lambda: P.matmul(out=PS[cb][:kn, :], lhsT=ones_b[:, :kn], rhs=Sacc[u][gs][:, :],
                                                         start=False, stop=True),
                                  reads=["ones_b", f"Sacc{u}_{gs}"], writes=[f"ps{cb}"])
                        fw.op("act", lambda: A.activation(out=ww[u][s3][:kn, :], in_=PS[cb][:kn, :], func=AF.Exp, scale=-1.0),
                              reads=[f"ps{cb}"], writes=[f"ww{u}_{s3}"])
                        fw.op("dve", lambda: V.tensor_tensor(out=aa[u][s3][:kn, :], in0=ee[u][s3][:kn, :], in1=ww[u][s3][:kn, :],
                                                             op=ALU.mult),
                              reads=[f"ee{u}_{s3}", f"ww{u}_{s3}"], writes=[f"aa{u}_{s3}"])
                        if j >= 0:
                            if first:
                                fw.op("dve", lambda: V.tensor_copy(out=Sacc[u][gs][:, :], in_=spb[u][s3][:, :]),
                                      reads=[f"spb{u}_{s3}"], writes=[f"Sacc{u}_{gs}"])
                            else:
                                fw.op("dve", lambda: V.tensor_tensor(out=Sacc[u][gs][:, :], in0=Sacc[u][gs][:, :],
                                                                     in1=spb[u][s3][:, :], op=ALU.add),
                                      reads=[f"spb{u}_{s3}", f"Sacc{u}_{gs}"], writes=[f"Sacc{u}_{gs}"])

                def stage_av(i):
                    j, a_ = blocks[i]
                    t = tctr[0] + i
                    s3 = t % NB
                    kn = 16 if j < 0 else 128
                    for u in range(2):
                        po = 64 * u
                        lhs = vmeta[hs][:, po:po + 64] if j < 0 else vts[hs][:, j, po:po + 64]
                        fw.op("pe", lambda: P.matmul(out=PS[6][po:po + 64, :], lhsT=lhs, rhs=aa[u][s3][:kn, :],
                                                     start=(i == 0), stop=(i == nb - 1), skip_group_check=True),
                              reads=[f"vts{hs}", f"vmeta{hs}", f"aa{u}_{s3}"], writes=["ps6"], signal=(u == 1 and i == nb - 1))

                stage_z(0)
                if nb > 1:
                    stage_z(1)
                stage_c(0)
                for i in range(nb):
                    if i + 2 < nb:
                        stage_z(i + 2)
                    if i + 1 < nb:
                        stage_c(i + 1)
                    stage_av(i)
                tctr[0] += nb
                fw.op("act", lambda: A.activation(out=osq2[:, :], in_=PS[6][:, :], func=AF.Square), reads=["ps6"], writes=["osq2"])
                fw.op("pe", lambda: P.matmul(out=PS[7][:, :], lhsT=ones2[:, :], rhs=osq2[:, :], start=True, stop=True),
                      reads=["osq2", "ones2"], writes=["ps7"])
                fw.op("act", lambda: A.activation(out=rs2[:, :], in_=PS[7][:, :], func=AF.Ln, scale=1.0 / 64, bias=epsc[:, :]),
                      reads=["ps7"], writes=["rs2"])
                fw.op("act", lambda: A.activation(out=rs2[:, :], in_=rs2[:, :], func=AF.Exp, scale=-0.5), reads=["rs2"], writes=["rs2"])
                fw.op("dve", lambda: V.scalar_tensor_tensor(out=mo[gs][:, :], in0=PS[6][:, :], scalar=gsb[:, hp:hp + 1], in1=rs2[:, :],
                                                            op0=ALU.mult, op1=ALU.mult),
                      reads=["ps6", "gsb", "rs2"], writes=[f"mo{gs}"])
                fw.dma(f"mo{gs}", MIXT[hp * 128:(hp + 1) * 128, g * 512:(g + 1) * 512], mo[gs][:, :], reads=[f"mo{gs}"], writes=["MIXT"])
        fw.barrier()

    if STOP < 3:
        return nc
    TG = 256
    with ExitStack() as st:
        wout = sb(st, "wout", [128, 8, D], BF16)
        wq = sb(st, "wq", [128, 8, D], BF16)
        g2b = sb(st, "g2b", [128, D], F32)
        gfb = sb(st, "gfb", [128, D], F32)
        fw.dma("c3", g2b[:, :], norm2_g[0:1, :].partition_broadcast(128).rearrange("p o n -> p (o n)"), writes=["g2b"])
        fw.dma("c4", gfb[:, :], norm_f_g[0:1, :].partition_broadcast(128).rearrange("p o n -> p (o n)"), writes=["gfb"])
        iota_b = sb(st, "iota_b", [128, 16, 128], BF16)
        fw.op("dve", lambda: V.tensor_copy(out=iota_b[:, :, :], in_=iotc[:, :].unsqueeze(1).to_broadcast([128, 16, 128])),
              reads=["iotc"], writes=["iota_b"])
        iota16 = iotc[:, 0:16]
        skb = sb(st, "skb", [128, 8, 256], BF16)
        with ExitStack() as st2:
            wstg = [sb(st2, f"wstg3{i}", [128, D], F32) for i in range(2)]
            for wi, (src, dst, nm) in enumerate(((w_out, wout, "wout"), (w_peer_q, wq, "wq"))):
                for k in range(8):
                    s = k % 2
                    fw.dma(f"wstg3{s}", wstg[s][:, :], src[k * 128:(k + 1) * 128, :], writes=[f"wstg3{s}"])
                    if k % 2 == 0:
                        fw.op("act", lambda: A.copy(out=dst[:, k, :], in_=wstg[s][:, :]), reads=[f"wstg3{s}"], writes=[nm])
                    else:
                        fw.op("dve", lambda: V.tensor_copy(out=dst[:, k, :], in_=wstg[s][:, :]), reads=[f"wstg3{s}"], writes=[nm])
            skst = sb(st2, "skst", [128, 8, 128], F32)
            fw.dma("c1", skst[:, :, 0:64], sub_keys1.rearrange("h k d -> k h d"), writes=["skst"], allow_slow_non_contiguous=True)
            fw.dma("c2", skst[:, :, 64:128], sub_keys2.rearrange("h k d -> k h d"), writes=["skst"], allow_slow_non_contiguous=True)
            fw.op("dve", lambda: V.memset(skb[:, :, :], 0.0), writes=["skb"])
            for h in range(8):
                fw.op("pe", lambda: P.transpose(out=PS[0][:, 0:128], in_=skst[:, h, :], identity=ident_f[:, :]),
                      reads=["skst"], writes=["ps0"])
                fw.op("dve", lambda: V.tensor_copy(out=skb[0:64, h, 0:128], in_=PS[0][0:64, 0:128]), reads=["ps0"], writes=["skb"])
                fw.op("dve", lambda: V.tensor_copy(out=skb[64:128, h, 128:256], in_=PS[0][64:128, 0:128]), reads=["ps0"], writes=["skb"])
        fw.barrier()

        mixs = sb(st, "mixs", [128, 8, TG], BF16)
        xs3 = sb(st, "xs3", [128, D], F32)
        h1 = sb(st, "h1", [128, 2, 2, D], F32)
        ss = sb(st, "ss3", [128, 1], F32)
        rs = sb(st, "rs3", [128, 1], F32)
        ss4 = sb(st, "ss4", [128, 1], F32)
        rs4 = sb(st, "rs4", [128, 1], F32)
        hn2 = sb(st, "hn2", [128, D], BF16)
        hn2T = sb(st, "hn2T", [128, 2, 8, TG], BF16)
        qpT = mixs
        ssb = sb(st, "ssb", [128, 4, 256], F32)
        wk = sb(st, "wk", [128, 256], F32)
        vv = sb(st, "vv", [128, 8, 2, 16], F32)
        ix = sb(st, "ix", [128, 8, 2, 16], U32)
        ixf = sb(st, "ixf", [128, 8, 2, 16], F32)
        cand = sb(st, "cand", [128, 8, 16, 16], F32)
        cc = sb(st, "cc", [128, 8, 16], F32)
        pos = sb(st, "pos", [128, 8, 16], U32)
        pa = sb(st, "pa", [128, 8, 16], U32)
        pb = sb(st, "pb", [128, 8, 16], U32)
        paf = sb(st, "paf", [128, 8, 16], F32)
        pbf = sb(st, "pbf", [128, 8, 16], F32)
        sel = cand
        Isel = sb(st, "Isel", [128, 128], F32)
        Jsel = sb(st, "Jsel", [128, 128], F32)
        gte = sb(st, "gte", [128, 8, 16], F32)
        zz = sb(st, "zz", [128, 8], F32)
        gate = sb(st, "gate", [128, 128], F32)
        ITb = sb(st, "ITb", [128, 2, TG], BF16)
        JTb = sb(st, "JTb", [128, 2, TG], BF16)
        GTb = sb(st, "GTb", [128, 2, TG], BF16)
        OA = [sb(st, f"OA{i}", [128, 16, 128], BF16) for i in range(2)]
        OB = [sb(st, f"OB{i}", [128, 16, 128], BF16) for i in range(2)]
        Gs = sb(st, "Gs", [128, 128, TG], BF16)
        utc = [sb(st, f"utc{i}", [128, 8, 128], BF16) for i in range(4)]
        vbc = [sb(st, f"vbc{i}", [128, D], BF16) for i in range(4)]
        gl = [sb(st, f"gl{i}", [128, TG], BF16) for i in range(2)]
        ga = [sb(st, f"ga{i}", [128, TG], BF16) for i in range(2)]
        h2 = xs3
        fw.barrier()

        def selection(grp):
            sl = grp % 2
            c0 = grp * TG
            fw.dma("mixs", mixs[:, :, :], MIXT[:, c0:c0 + TG].rearrange("(k p) t -> p k t", p=128), writes=["mixs"])
            for tl in range(2):
                fw.dma("xs3", xs3[:, :], x[c0 + tl * 128:c0 + (tl + 1) * 128, :], writes=["xs3"])
                for hf in range(2):
                    bk = 6 + hf
                    for k in range(8):
                        fw.op("pe", lambda: P.matmul(out=PS[bk][:, :], lhsT=mixs[:, k, tl * 128:(tl + 1) * 128],
                                                     rhs=wout[:, k, hf * 512:(hf + 1) * 512], start=(k == 0), stop=(k == 7)),
                              reads=["mixs", "wout"], writes=[f"ps{bk}"], signal=(k == 7))
                    fw.op("dve", lambda: V.tensor_tensor(out=h1[:, sl, tl, hf * 512:(hf + 1) * 512], in0=PS[bk][:, :],
                                                         in1=xs3[:, hf * 512:(hf + 1) * 512], op=ALU.add),
                          reads=[f"ps{bk}", "xs3"], writes=[f"h1_{sl}_{tl}"])
                    yield
                fw.op("act", lambda: A.activation(out=hn2[:, :], in_=h1[:, sl, tl, :], func=AF.Square, accum_out=ss[:, :]),
                      reads=[f"h1_{sl}_{tl}"], writes=["hn2", "ss3"])
                rstd_from_ss(ss, rs, D, "ss3", "rs3")
                fw.op("dve", lambda: V.scalar_tensor_tensor(out=hn2[:, :], in0=h1[:, sl, tl, :], scalar=rs[:, :], in1=g2b[:, :],
                                                            op0=ALU.mult, op1=ALU.mult),
                      reads=[f"h1_{sl}_{tl}", "rs3", "g2b"], writes=["hn2"])
                yield
                ptr = PS[6][:, :].bitcast(BF16)
                for k in range(8):
                    fw.op("pe", lambda: P.transpose(out=ptr[:, k * 128:(k + 1) * 128], in_=hn2[:, k * 128:(k + 1) * 128],
                                                    identity=ident_b[:, :]),
                          reads=["hn2"], writes=["ps6"], signal=(k == 7))
                fw.op("act", lambda: A.copy(out=hn2T[:, sl, :, tl * 128:(tl + 1) * 128], in_=ptr.rearrange("p (k t) -> p k t", k=8)),
                      reads=["ps6"], writes=[f"hn2T{sl}"])
                yield
            for h in range(8):
                bk = 6 + h % 2
                for k in range(8):
                    fw.op("pe", lambda: P.matmul(out=PS[bk][:, 0:TG], lhsT=wq[:, k, h * 128:(h + 1) * 128], rhs=hn2T[:, sl, k, :],
                                                 start=(k == 0), stop=(k == 7)),
                          reads=["wq", f"hn2T{sl}"], writes=[f"ps{bk}"], signal=(k == 7))
                if h % 2 == 0:
                    fw.op("act", lambda: A.copy(out=qpT[:, h, :], in_=PS[bk][:, 0:TG]), reads=[f"ps{bk}"], writes=["mixs"])
                else:
                    fw.op("dve", lambda: V.tensor_copy(out=qpT[:, h, :], in_=PS[bk][:, 0:TG]), reads=[f"ps{bk}"], writes=["mixs"])
                yield
            for tl in range(2):
                tsl = slice(tl * 128, (tl + 1) * 128)
                for hq in range(2):
                    for hp in range(2):
                        bk = 6 + hp
                        for hh in range(2):
                            h = hq * 4 + hp * 2 + hh
                            fw.op("pe", lambda: P.matmul(out=PS[bk][:, hh * 256:(hh + 1) * 256], lhsT=qpT[:, h, tsl], rhs=skb[:, h, :],
                                                         start=True, stop=True),
                                  reads=["mixs", "skb"], writes=[f"ps{bk}"], signal=(hh == 1))
                        fw.op("act", lambda: A.copy(out=ssb[:, hp * 2:hp * 2 + 2, :].rearrange("p a b -> p (a b)"), in_=PS[bk][:, :]),
                              reads=[f"ps{bk}"], writes=["ssb"])
                    yield
                    for h4 in range(4):
                        h = hq * 4 + h4
                        for hf in range(2):
                            src = ssb[:, h4, hf * 128:(hf + 1) * 128]
                            fw.op("dve", lambda: V.max(out=vv[:, h, hf, 0:8], in_=src), reads=["ssb"], writes=["vv"])
                            fw.op("dve", lambda: V.match_replace(out=wk[:, 0:128], in_to_replace=vv[:, h, hf, 0:8], in_values=src,
                                                                 imm_value=NEG), reads=["ssb", "vv"], writes=["wk"])
                            fw.op("dve", lambda: V.max(out=vv[:, h, hf, 8:16], in_=wk[:, 0:128]), reads=["wk"], writes=["vv"])
                            fw.op("dve", lambda: V.max_index(out=ix[:, h, hf, 0:8], in_max=vv[:, h, hf, 0:8], in_values=src),
                                  reads=["ssb", "vv"], writes=["ix"])
                            fw.op("dve", lambda: V.max_index(out=ix[:, h, hf, 8:16], in_max=vv[:, h, hf, 8:16], in_values=src),
                                  reads=["ssb", "vv"], writes=["ix"])
                            yield
                fw.op("dve", lambda: V.tensor_copy(out=ixf[:, :, :, :], in_=ix[:, :, :, :]), reads=["ix"], writes=["ixf"])
                fw.op("dve", lambda: V.tensor_tensor(out=cand[:, :, :, :], in0=vv[:, :, 0, :].unsqueeze(3).to_broadcast([128, 8, 16, 16]),
                                                     in1=vv[:, :, 1, :].unsqueeze(2).to_broadcast([128, 8, 16, 16]), op=ALU.add),
                      reads=["vv"], writes=["cand"])
                yield
                for h in range(8):
                    src = cand[:, h, :, :].rearrange("p a b -> p (a b)")
                    fw.op("dve", lambda: V.max(out=cc[:, h, 0:8], in_=src), reads=["cand"], writes=["cc"])
                    fw.op("dve", lambda: V.match_replace(out=wk[:, :], in_to_replace=cc[:, h, 0:8], in_values=src, imm_value=NEG),
                          reads=["cand", "cc"], writes=["wk"])
                    fw.op("dve", lambda: V.max(out=cc[:, h, 8:16], in_=wk[:, :]), reads=["wk"], writes=["cc"])
                    fw.op("dve", lambda: V.max_index(out=pos[:, h, 0:8], in_max=cc[:, h, 0:8], in_values=src), reads=["cand", "cc"], writes=["pos"])
                    fw.op("dve", lambda: V.max_index(out=pos[:, h, 8:16], in_max=cc[:, h, 8:16], in_values=src), reads=["cand", "cc"], writes=["pos"])
                    yield
                fw.op("dve", lambda: V.tensor_single_scalar(out=pa[:, :, :], in_=pos[:, :, :], scalar=4, op=ALU.logical_shift_right),
                      reads=["pos"], writes=["pa"])
                fw.op("dve", lambda: V.tensor_single_scalar(out=pb[:, :, :], in_=pos[:, :, :], scalar=15, op=ALU.bitwise_and),
                      reads=["pos"], writes=["pb"])
                fw.op("dve", lambda: V.tensor_copy(out=paf[:, :, :], in_=pa[:, :, :]), reads=["pa"], writes=["paf"])
                fw.op("dve", lambda: V.tensor_copy(out=pbf[:, :, :], in_=pb[:, :, :]), reads=["pb"], writes=["pbf"])
                yield
                for (pf, hf, dst, nm) in ((paf, 0, Isel, "Isel"), (pbf, 1, Jsel, "Jsel")):
                    fw.op("dve", lambda: V.tensor_tensor(out=sel[:, :, :, :],
                                                         in0=iota16.unsqueeze(1).unsqueeze(1).to_broadcast([128, 8, 16, 16]),
                                                         in1=pf[:, :, :].unsqueeze(3).to_broadcast([128, 8, 16, 16]), op=ALU.is_equal),
                          reads=["iotc", "paf", "pbf"], writes=["cand"])
                    yield
                    fw.op("dve", lambda: V.tensor_tensor(out=sel[:, :, :, :], in0=sel[:, :, :, :],
                                                         in1=ixf[:, :, hf, :].unsqueeze(2).to_broadcast([128, 8, 16, 16]), op=ALU.mult),
                          reads=["cand", "ixf"], writes=["cand"])
                    yield
                    fw.op("dve", lambda: V.tensor_reduce(out=dst[:, :], in_=sel[:, :, :, :].rearrange("p h r a -> p (h r) a"),
                                                         axis=AX.X, op=ALU.add),
                          reads=["cand"], writes=[nm])
                    yield
                fw.op("dve", lambda: V.tensor_tensor(out=gte[:, :, :], in0=cc[:, :, :], in1=cc[:, :, 0:1].to_broadcast([128, 8, 16]),
                                                     op=ALU.subtract), reads=["cc"], writes=["gte"])
                fw.op("act", lambda: A.activation(out=gte[:, :, :], in_=gte[:, :, :], func=AF.Exp), reads=["gte"], writes=["gte"])
                fw.op("dve", lambda: V.tensor_reduce(out=zz[:, :], in_=gte[:, :, :], axis=AX.X, op=ALU.add), reads=["gte"], writes=["zz"])
                fw.op("dve", lambda: V.reciprocal(out=zz[:, :], in_=zz[:, :]), reads=["zz"], writes=["zz"])
                fw.op("dve", lambda: V.tensor_tensor(out=gate[:, :].rearrange("p (h r) -> p h r", h=8), in0=gte[:, :, :],
                                                     in1=zz[:, :].unsqueeze(2).to_broadcast([128, 8, 16]), op=ALU.mult),
                      reads=["gte", "zz"], writes=["gate"])
                yield
                for qi, (srcT, nm, dstT) in enumerate(((Isel, "Isel", ITb), (Jsel, "Jsel", JTb), (gate, "gate", GTb))):
                    fw.op("pe", lambda: P.transpose(out=PS[7][:, qi * 128:(qi + 1) * 128], in_=srcT[:, :], identity=ident_f[:, :]),
                          reads=[nm], writes=["ps7"])
                    fw.op("act", lambda: A.copy(out=dstT[:, sl, tsl], in_=PS[7][:, qi * 128:(qi + 1) * 128]), reads=["ps7"],
                          writes=[f"{nm}T{sl}"])
                yield

        def gbuild_half(grp, half):
            sl = grp % 2
            gk = "GsLo" if half == 0 else "GsHi"
            i0 = half * 64

            def prep(sbt):
                o = sbt % 2
                t0 = sbt * 16
                fw.op("dve", lambda: V.tensor_tensor(out=OB[o][:, :, :], in0=iota_b[:, :, :],
                                                     in1=JTb[:, sl, t0:t0 + 16].unsqueeze(2).to_broadcast([128, 16, 128]), op=ALU.is_equal),
                      reads=["iota_b", f"JselT{sl}"], writes=[f"OB{o}"])
                fw.op("dve", lambda: V.tensor_tensor(out=OA[o][:, :, 0:64], in0=iota_b[:, :, i0:i0 + 64],
                                                     in1=ITb[:, sl, t0:t0 + 16].unsqueeze(2).to_broadcast([128, 16, 64]), op=ALU.is_equal),
                      reads=["iota_b", f"IselT{sl}"], writes=[f"OA{o}"])
                fw.op("pool", lambda: G.tensor_tensor(out=OA[o][:, :, 0:64], in0=OA[o][:, :, 0:64],
                                                      in1=GTb[:, sl, t0:t0 + 16].unsqueeze(2).to_broadcast([128, 16, 64]), op=ALU.mult),
                      reads=[f"OA{o}", f"gateT{sl}"], writes=[f"OA{o}"])

            def mm(sbt):
                o = sbt % 2
                t0 = sbt * 16
                for q in range(2):
                    bk = 6 + q
                    for tt in range(8):
                        ti = q * 8 + tt
                        fw.op("pe", lambda: P.matmul(out=PS[bk][:, tt * 64:(tt + 1) * 64], lhsT=OB[o][:, ti, :], rhs=OA[o][:, ti, 0:64],
                                                     start=True, stop=True),
                              reads=[f"OA{o}", f"OB{o}"], writes=[f"ps{bk}"], signal=(tt == 7))
                    tq = t0 + q * 8
                    src = PS[bk][:, :].rearrange("p (t i) -> p i t", t=8)
                    fw.op("act", lambda: A.copy(out=Gs[:, i0:i0 + 64, tq:tq + 8], in_=src), reads=[f"ps{bk}"], writes=[gk])

            nsb = TG // 16
            prep(0)
            yield
            for sbt in range(nsb):
                if sbt + 1 < nsb:
                    prep(sbt + 1)
                    yield
                if half == 1:
                    yield
                mm(sbt)
                yield

        def eloop(grp, hi_gen, sel_gen, lo_gen, preloaded, preload_next):
            sl = grp % 2

            def ld(i):
                s = i % 4
                fw.dma(f"utc{s}", utc[s][:, :, :], UT[i], writes=[f"utc{s}"])
                fw.dma(f"vbc{s}", vbc[s][:, :], VB[i * 128:(i + 1) * 128, :], writes=[f"vbc{s}"])

            def st_u(i):
                s = i % 4
                hb = i % 2
                for k in range(8):
                    fw.op("pe", lambda: P.matmul(out=PS[4 + hb][:, 0:TG], lhsT=utc[s][:, k, :], rhs=hn2T[:, sl, k, :],
                                                 start=(k == 0), stop=(k == 7)),
                          reads=[f"utc{s}", f"hn2T{sl}"], writes=[f"ps{4 + hb}"], signal=(k == 7))
                fw.op("act", lambda: A.activation(out=gl[hb][:, :], in_=PS[4 + hb][:, 0:TG], func=AF.Gelu),
                      reads=[f"ps{4 + hb}"], writes=[f"gl{hb}"])
                fw.op("pool", lambda: G.tensor_tensor(out=ga[hb][:, :], in0=gl[hb][:, :], in1=Gs[:, i, :], op=ALU.mult),
                      reads=[f"gl{hb}", "GsLo" if i < 64 else "GsHi"], writes=[f"ga{hb}"])

            def st_v(i):
                s = i % 4
                hb = i % 2
                for tl in range(2):
                    for hf in range(2):
                        bk = 2 * tl + hf
                        fw.op("pe", lambda: P.matmul(out=PS[bk][:, :], lhsT=ga[hb][:, tl * 128:(tl + 1) * 128],
                                                     rhs=vbc[s][:, hf * 512:(hf + 1) * 512], start=(i == 0), stop=(i == 127)),
                              reads=[f"ga{hb}", f"vbc{s}"], writes=[f"ps{bk}"], signal=(tl == 1 and hf == 1))

            def drain(gen):
                if gen is not None:
                    for _ in gen:
                        pass

            def step(gen):
                if gen is None:
                    return True
                try:
                    next(gen)
                    return False
                except StopIteration:
                    return True

            hi_done = hi_gen is None
            sel_done = sel_gen is None
            if not preloaded:
                ld(0)
                ld(1)
                ld(2)
            st_u(0)
            for i in range(128):
                if i + 3 < 128:
                    ld(i + 3)
                elif preload_next:
                    ld(i + 3 - 128)
                if i + 1 < 128:
                    if i + 1 == 64 and not hi_done:
                        drain(hi_gen)
                        hi_done = True
                    st_u(i + 1)
                st_v(i)
                if i >= 1:
                    if not hi_done:
                        hi_done = step(hi_gen)
                    if not sel_done:
                        sel_done = step(sel_gen)
                    elif i >= 63 and lo_gen is not None:
                        step(lo_gen)
            drain(sel_gen)
            drain(lo_gen)

        def epilogue(grp):
            sl = grp % 2
            c0 = grp * TG
            for tl in range(2):
                for hf in range(2):
                    bk = 2 * tl + hf
                    fw.op("dve", lambda: V.tensor_tensor(out=h2[:, hf * 512:(hf + 1) * 512], in0=PS[bk][:, :],
                                                         in1=h1[:, sl, tl, hf * 512:(hf + 1) * 512], op=ALU.add),
                          reads=[f"ps{bk}", f"h1_{sl}_{tl}"], writes=["xs3"])
                fw.op("act", lambda: A.activation(out=OA[0][:, 0:8, :].rearrange("p a b -> p (a b)"), in_=h2[:, :], func=AF.Square,
                                                  accum_out=ss4[:, :]),
                      reads=["xs3"], writes=["OA0", "ss4"])
                rstd_from_ss(ss4, rs4, D, "ss4", "rs4")
                fw.op("dve", lambda: V.scalar_tensor_tensor(out=h2[:, :], in0=h2[:, :], scalar=rs4[:, :], in1=gfb[:, :],
                                                            op0=ALU.mult, op1=ALU.mult),
                      reads=["xs3", "rs4", "gfb"], writes=["xs3"])
                fw.dma("xs3", out[c0 + tl * 128:c0 + (tl + 1) * 128, :], h2[:, :], reads=["xs3"], writes=["out"])

        ngrp = NR // TG
        for _ in selection(0):
            pass
        for _ in gbuild_half(0, 0):
            pass
        for _ in gbuild_half(0, 1):
            pass
        for grp in range(ngrp):
            last = grp + 1 >= ngrp
            eloop(grp,
                  gbuild_half(grp, 1) if grp > 0 else None,
                  None if last else selection(grp + 1),
                  None if last else gbuild_half(grp + 1, 0),
                  grp > 0, not last)
            epilogue(grp)
        fw.barrier()
    glob.close()
    return nc


_CACHE = {}


def kernel(**inputs):
    xf = np.ascontiguousarray(np.asarray(inputs["x"], dtype=np.float32))
    B, NR, _ = xf.shape
    assert B == 8 and NR % 512 == 0
    if NR not in _CACHE:
        _CACHE[NR] = build(NR)
    nc = _CACHE[NR]
    f = lambda k: np.ascontiguousarray(np.asarray(inputs[k], dtype=np.float32))
    shared = {
        "meta_tokens": f("meta_tokens"),
        "norm1_g": f("norm1_g").reshape(1, D),
        "w_in": f("w_in").reshape(D, INC),
        "w_gate_up": f("w_gate_up").reshape(16, 256),
        "b_gate_up": f("b_gate_up").reshape(1, 256),
        "sb_norm_g": f("sb_norm_g").reshape(1, 512),
        "gla_norm_g": f("gla_norm_g").reshape(1, 512),
        "w_out": f("w_out").reshape(D, D),
        "norm2_g": f("norm2_g").reshape(1, D),
        "w_peer_q": f("w_peer_q").reshape(D, D),
        "sub_keys1": f("sub_keys1").reshape(8, 128, 64),
        "sub_keys2": f("sub_keys2").reshape(8, 128, 64),
        "expert_u": f("expert_u").reshape(16384, D),
        "expert_v": f("expert_v").reshape(16384, D),
        "norm_f_g": f("norm_f_g").reshape(1, D),
    }
    in_maps = [dict(shared, x=xf[c]) for c in range(8)]
    res = run_bass_kernel_spmd(nc, in_maps, core_ids=list(range(8)))
    return np.stack([np.asarray(res.results[c]["out"], dtype=np.float32).reshape(NR, D) for c in range(8)], axis=0)
```

```python
from contextlib import ExitStack
import numpy as np
import concourse.bass as bass
import concourse.mybir as mybir
from concourse.bass_utils import run_bass_kernel_spmd

F32 = mybir.dt.float32
BF16 = mybir.dt.bfloat16
U32 = mybir.dt.uint32
ALU = mybir.AluOpType
AF = mybir.ActivationFunctionType
AX = mybir.AxisListType

D = 1024
NMETA = 16
EPS = 1e-6
INC = 3088
NEG = -1e30


class FW:
    def __init__(self, nc):
        self.nc = nc
        self.eng = {"pe": nc.tensor, "act": nc.scalar, "dve": nc.vector, "pool": nc.gpsimd, "sp": nc.sync}
        self.sem = {}
        self.cnt = {}
        for e in ("pe", "act", "dve", "pool"):
            self.sem[e] = nc.alloc_semaphore(f"s_{e}")
            self.cnt[e] = 0
        self.seen = {}
        self.lastw = {}
        self.readers = {}
        self.dsem = {}
        self.pending = {e: False for e in ("pe", "act", "dve", "pool")}

    def _wait(self, engname, tok):
        if tok is None:
            return
        semname, sem, val = tok
        if semname == engname and val > self.cnt[engname]:
            return
        k = (engname, semname)
        if self.seen.get(k, 0) >= val:
            return
        self.seen[k] = val
        self.eng[engname].wait_ge(sem, val)

    def deps(self, engname, reads, writes):
        for b in reads:
            self._wait(engname, self.lastw.get(b))
        for b in writes:
            self._wait(engname, self.lastw.get(b))
            for t in self.readers.get(b, ()):
                self._wait(engname, t)

    def commit(self, tok, reads, writes):
        for b in reads:
            self.readers.setdefault(b, []).append(tok)
        for b in writes:
            self.lastw[b] = tok
            self.readers[b] = []

    @staticmethod
    def _norm(reads, writes):
        isps = lambda b: b.startswith("ps") and len(b) > 2 and b[2].isdigit()
        r2 = [b for b in reads if not isps(b)]
        w2 = [b[:3] if isps(b) else b for b in writes] + [b[:3] for b in reads if isps(b)]
        return r2, w2

    def op(self, engname, fn, reads=(), writes=(), signal=True):
        reads, writes = self._norm(reads, writes)
        self.deps(engname, reads, writes)
        ins = fn()
        if signal:
            self.cnt[engname] += 1
            ins.then_inc(self.sem[engname], 1)
            tok = (engname, self.sem[engname], self.cnt[engname])
            self.pending[engname] = False
        else:
            tok = (engname, self.sem[engname], self.cnt[engname] + 1)
            self.pending[engname] = True
        self.commit(tok, reads, writes)
        return tok

    def dma(self, slot, out, in_, reads=(), writes=(), q="sp", **kw):
        if slot not in self.dsem:
            self.dsem[slot] = [self.nc.alloc_semaphore(f"d_{slot}"), 0]
        reads, writes = self._norm(reads, writes)
        self.deps(q, reads, writes)
        ins = self.eng[q].dma_start(out=out, in_=in_, **kw)
        d = self.dsem[slot]
        d[1] += 16
        ins.then_inc(d[0], 16)
        tok = ("d_" + slot, d[0], d[1])
        self.commit(tok, reads, writes)
        return tok

    def barrier(self):
        assert not any(self.pending.values())
        toks = [(e, self.sem[e], self.cnt[e]) for e in ("pe", "act", "dve", "pool") if self.cnt[e] > 0]
        toks += [("d_" + s, d[0], d[1]) for s, d in self.dsem.items()]
        for e in ("pe", "act", "dve", "pool", "sp"):
            for t in toks:
                self._wait(e, t)
        self.lastw.clear()
        self.readers.clear()


STOP = 99
SKIP_SETUP = False
NCH_DBG = 0
DBG = 'uvd'
P1STOP = 99


def build(NR):
    NG = NR // 512
    NT = NR // 128
    L = NR + NMETA
    nc = bass.Bass("TRN2", target_bir_lowering=False)
    dt = lambda n, s, k="ExternalInput", d=F32: nc.dram_tensor(n, s, d, kind=k).ap()
    x = dt("x", [NR, D])
    meta = dt("meta_tokens", [NMETA, D])
    norm1_g = dt("norm1_g", [1, D])
    w_in = dt("w_in", [D, INC])
    w_gate_up = dt("w_gate_up", [16, 256])
    b_gate_up = dt("b_gate_up", [1, 256])
    sb_norm_g = dt("sb_norm_g", [1, 512])
    gla_norm_g = dt("gla_norm_g", [1, 512])
    w_out = dt("w_out", [D, D])
    norm2_g = dt("norm2_g", [1, D])
    w_peer_q = dt("w_peer_q", [D, D])
    sub_keys1 = dt("sub_keys1", [8, 128, 64])
    sub_keys2 = dt("sub_keys2", [8, 128, 64])
    expert_u = dt("expert_u", [16384, D])
    expert_v = dt("expert_v", [16384, D])
    norm_f_g = dt("norm_f_g", [1, D])
    out = dt("out", [NR, D], "ExternalOutput")
    QT = nc.dram_tensor("QT", [512, NR], BF16).ap()
    KT = nc.dram_tensor("KT", [512, L], BF16).ap()
    VT = nc.dram_tensor("VT", [L, 512], BF16).ap()
    MIXT = nc.dram_tensor("MIXT", [D, NR], BF16).ap()
    UT = nc.dram_tensor("UT", [128, 128, 8, 128], BF16).ap()
    VB = nc.dram_tensor("VB", [16384, D], BF16).ap()

    fw = FW(nc)
    V, A, P, G, SP = nc.vector, nc.scalar, nc.tensor, nc.gpsimd, nc.sync
    PS = [nc.alloc_psum_tensor(f"ps{i}", [128, 512], F32) for i in range(8)]

    glob = ExitStack()

    def sb(stack, name, shape, dtype):
        return stack.enter_context(nc.sbuf_tensor(name, shape, dtype))

    ident_f = sb(glob, "ident_f", [128, 128], F32)
    ident_b = sb(glob, "ident_b", [128, 128], BF16)
    ones_b = sb(glob, "ones_b", [128, 128], BF16)
    iot = sb(glob, "iot", [128, 128], F32)
    iotc = sb(glob, "iotc", [128, 128], F32)
    fw.op("pool", lambda: G.iota(iot[:, :], pattern=[[1, 128]], base=0, channel_multiplier=-1,
                                 allow_small_or_imprecise_dtypes=True), writes=["iot"])
    fw.op("pool", lambda: G.iota(iotc[:, :], pattern=[[1, 128]], base=0, channel_multiplier=0,
                                 allow_small_or_imprecise_dtypes=True), writes=["iotc"])
    fw.op("dve", lambda: V.tensor_single_scalar(out=ident_f[:, :], in_=iot[:, :], scalar=0.0, op=ALU.is_equal),
          reads=["iot"], writes=["ident_f"])
    fw.op("dve", lambda: V.tensor_copy(out=ident_b[:, :], in_=ident_f[:, :]), reads=["ident_f"], writes=["ident_b"])
    fw.op("dve", lambda: V.memset(ones_b[:, :], 1.0), writes=["ones_b"])

    def rstd_from_ss(ss, rs, n, key_ss, key_rs, npart=128):
        fw.op("act", lambda: A.activation(out=rs[:npart, :], in_=ss[:npart, :], func=AF.Ln, scale=1.0 / n, bias=epsc[:npart, :]),
              reads=[key_ss], writes=[key_rs])
        fw.op("act", lambda: A.activation(out=rs[:npart, :], in_=rs[:npart, :], func=AF.Exp, scale=-0.5),
              reads=[key_rs], writes=[key_rs])

    epsc = sb(glob, "epsc", [128, 1], F32)
    fw.op("dve", lambda: V.memset(epsc[:, :], EPS), writes=["epsc"])
    fw.barrier()

    if STOP < 1:
        return nc
    with ExitStack() as st:
        win = sb(st, "win", [128, 8, INC], BF16)
        wstg = [sb(st, f"wstg{i}", [128, INC], F32) for i in range(2)]
        for k in range(8):
            s = k % 2
            fw.dma(f"wstg{s}", wstg[s][:, :], w_in[k * 128:(k + 1) * 128, :], writes=[f"wstg{s}"])
            e = ("act", "dve", "pool")[k % 3]
            if e == "act":
                fw.op("act", lambda: A.copy(out=win[:, k, :], in_=wstg[s][:, :]), reads=[f"wstg{s}"], writes=["win"])
            elif e == "dve":
                fw.op("dve", lambda: V.tensor_copy(out=win[:, k, :], in_=wstg[s][:, :]), reads=[f"wstg{s}"], writes=["win"])
            else:
                fw.op("pool", lambda: G.tensor_copy(out=win[:, k, :], in_=wstg[s][:, :]), reads=[f"wstg{s}"], writes=["win"])
        g1b = sb(st, "g1b", [128, D], F32)
        fw.dma("c1", g1b[:, :], norm1_g[0:1, :].partition_broadcast(128).rearrange("p o n -> p (o n)"), writes=["g1b"])
        wgu_f = sb(st, "wgu_f", [16, 256], F32)
        wgu = sb(st, "wgu", [16, 256], BF16)
        bgu_f = sb(st, "bgu_f", [1, 256], F32)
        bgu = sb(st, "bgu", [1, 256], BF16)
        fw.dma("c2", wgu_f[:, :], w_gate_up[:, :], writes=["wgu_f"])
        fw.dma("c3", bgu_f[:, :], b_gate_up[:, :], writes=["bgu_f"])
        fw.op("dve", lambda: V.tensor_copy(out=wgu[:, :], in_=wgu_f[:, :]), reads=["wgu_f"], writes=["wgu"])
        fw.op("dve", lambda: V.tensor_copy(out=bgu[:, :], in_=bgu_f[:, :]), reads=["bgu_f"], writes=["bgu"])
        ggla = sb(st, "ggla", [128, 4], F32)
        fw.dma("c4", ggla[:, :], gla_norm_g.rearrange("o (h e) -> e (o h)", e=128), writes=["ggla"],
               allow_slow_non_contiguous=True)
        triI = sb(st, "triI", [128, 128], F32)
        strA = sb(st, "strA", [128, 128], F32)
        m01 = sb(st, "m01", [128, 128], F32)
        fw.op("dve", lambda: V.tensor_scalar(out=triI[:, :], in0=iot[:, :], scalar1=0.0, scalar2=-1.0 / 16, op0=ALU.is_ge, op1=ALU.mult),
              reads=["iot"], writes=["triI"])
        fw.op("dve", lambda: V.tensor_scalar(out=strA[:, :], in0=iot[:, :], scalar1=0.0, scalar2=-1.0 / 16, op0=ALU.is_lt, op1=ALU.mult),
              reads=["iot"], writes=["strA"])
        fw.op("dve", lambda: V.tensor_single_scalar(out=m01[:, :], in_=iot[:, :], scalar=0.0, op=ALU.is_ge),
              reads=["iot"], writes=["m01"])

        xs = [sb(st, f"xs{i}", [128, D], F32) for i in range(2)]
        junk = sb(st, "junk", [128, D], BF16)
        ss = sb(st, "ss", [128, 1], F32)
        rs = sb(st, "rs", [128, 1], F32)
        hn = sb(st, "hn", [128, D], BF16)
        hnT = sb(st, "hnT", [128, 8, 512], BF16)
        qk_o = [sb(st, f"qko{i}", [128, 512], BF16) for i in range(2)]
        gqT = sb(st, "gqT", [64, 4, 512], F32)
        gkT = sb(st, "gkT", [64, 4, 512], F32)
        grS = sb(st, "grS", [128, 4, 512], F32)
        glrT = sb(st, "glrT", [16, 512], BF16)
        ones1 = sb(st, "ones1", [1, 128], BF16)
        fw.op("dve", lambda: V.memset(ones1[:, :], 1.0), writes=["ones1"])
        vtok = [sb(st, f"vtok{i}", [128, 512], BF16) for i in range(2)]
        gv = sb(st, "gv", [128, 512], BF16)
        gkt = sb(st, "gkt", [128, 256], F32)
        spl = sb(st, "spl", [128, 256], F32)
        eT = sb(st, "eT", [64, 4, 128], F32)
        enT = sb(st, "enT", [64, 4, 128], F32)
        qtl = sb(st, "qtl", [64, 4, 128], BF16)
        ktl = sb(st, "ktl", [64, 4, 128], BF16)
        ebd = sb(st, "ebd", [128, 256], F32)
        khat = sb(st, "khat", [128, 256], BF16)
        attn = sb(st, "attn", [128, 4, 128], BF16)
        state = sb(st, "state", [64, 4, 128], F32)
        stb = sb(st, "stb", [64, 4, 128], BF16)
        osq = sb(st, "osq", [128, 512], BF16)
        rso = sb(st, "rso", [128, 512], F32)
        o1 = sb(st, "o1", [128, 512], F32)
        mixg = [sb(st, f"mixg{i}", [128, 4, 128], BF16) for i in range(2)]
        fw.op("dve", lambda: V.memset(state[:, :, :], 0.0), writes=["state"])
        fw.op("dve", lambda: V.memset(stb[:, :, :], 0.0), writes=["stb"])

        if P1STOP == 0:
            fw.barrier()
            return nc
        ust = [sb(st, f"ust{i}", [128, D], F32) for i in range(2)]
        vst = [sb(st, f"vst{i}", [128, D], F32) for i in range(2)]
        utb = [sb(st, f"utb{i}", [128, 8, 128], BF16) for i in range(2)]
        vbb = [sb(st, f"vbb{i}", [128, D], BF16) for i in range(2)]

        def setup_gen():
            def load(c):
                s_ = c % 2
                fw.dma(f"ust{s_}", ust[s_][:, :], expert_u[c * 128:(c + 1) * 128, :], writes=[f"ust{s_}"])
                fw.dma(f"vst{s_}", vst[s_][:, :], expert_v[c * 128:(c + 1) * 128, :], writes=[f"vst{s_}"])

            def work(c):
                s_ = c % 2
                for hlf in range(2):
                    pt = PS[hlf]
                    for k4 in range(4):
                        k = hlf * 4 + k4
                        fw.op("pe", lambda: P.transpose(out=pt[:, k4 * 128:(k4 + 1) * 128], in_=ust[s_][:, k * 128:(k + 1) * 128],
                                                        identity=ident_f[:, :]),
                              reads=[f"ust{s_}"], writes=[f"ps{hlf}"], signal=(k4 == 3))
                    if hlf == 0:
                        fw.op("act", lambda: A.copy(out=utb[s_][:, 0:4, :].rearrange("p a b -> p (a b)"), in_=pt[:, :]),
                              reads=[f"ps{hlf}"], writes=[f"utb{s_}_0"])
                    else:
                        fw.op("dve", lambda: V.tensor_copy(out=utb[s_][:, 4:8, :].rearrange("p a b -> p (a b)"), in_=pt[:, :]),
                              reads=[f"ps{hlf}"], writes=[f"utb{s_}_1"])
                fw.op("pool", lambda: G.tensor_copy(out=vbb[s_][:, :], in_=vst[s_][:, :]), reads=[f"vst{s_}"], writes=[f"vbb{s_}"])

            def store(c):
                s_ = c % 2
                fw.dma(f"utb{s_}", UT[c], utb[s_][:, :, :], reads=[f"utb{s_}_0", f"utb{s_}_1"], writes=["UT"], q="pool")
                fw.dma(f"vbb{s_}", VB[c * 128:(c + 1) * 128, :], vbb[s_][:, :], reads=[f"vbb{s_}"], writes=["VB"], q="pool")

            nch = NCH_DBG if SKIP_SETUP else 128
            if nch == 0:
                return
            load(0)
            yield
            for c in range(nch):
                if c + 1 < nch:
                    load(c + 1)
                work(c)
                if c >= 1:
                    store(c - 1)
                yield
            store(nch - 1)
            yield

        sg = setup_gen()
        tile_ctr = [0]
        for g in range(-1, NG):
            n = NMETA if g < 0 else 512
            ntile = 1 if g < 0 else 4
            tn = NMETA if g < 0 else 128
            for tl in range(ntile):
                s = tile_ctr[0] % 2
                tile_ctr[0] += 1
                src = meta[:, :] if g < 0 else x[g * 512 + tl * 128: g * 512 + (tl + 1) * 128, :]
                fw.dma(f"xs{s}", xs[s][:tn, :], src, writes=[f"xs{s}"])
                fw.op("act", lambda: A.activation(out=junk[:tn, :], in_=xs[s][:tn, :], func=AF.Square, accum_out=ss[:tn, :]),
                      reads=[f"xs{s}"], writes=["junk", "ss"])
                rstd_from_ss(ss, rs, D, "ss", "rs", tn)
                fw.op("dve", lambda: V.scalar_tensor_tensor(out=hn[:tn, :], in0=xs[s][:tn, :], scalar=rs[:tn, :], in1=g1b[:tn, :],
                                                            op0=ALU.mult, op1=ALU.mult),
                      reads=[f"xs{s}", "rs", "g1b"], writes=["hn"])
                ptr = PS[0][:, :].bitcast(BF16)
                for k in range(8):
                    fw.op("pe", lambda: P.transpose(out=ptr[:, k * 128:k * 128 + tn], in_=hn[:tn, k * 128:(k + 1) * 128],
                                                    identity=ident_b[:tn, :tn]),
                          reads=["hn"], writes=["ps0"], signal=(k == 7))
                fw.op("act", lambda: A.copy(out=hnT[:, :, tl * 128:tl * 128 + tn],
                                            in_=ptr.rearrange("p (k t) -> p k t", k=8)[:, :, :tn]),
                      reads=["ps0"], writes=["hnT"])
            if P1STOP == 1:
                fw.barrier()
                return nc
            def fproj(col0, m, evac):
                for k in range(8):
                    fw.op("pe", lambda: P.matmul(out=PS[1][:m, :n], lhsT=win[:, k, col0:col0 + m], rhs=hnT[:, k, :n],
                                                 start=(k == 0), stop=(k == 7)),
                          reads=["win", "hnT"], writes=["ps1"], signal=(k == 7))
                evac(PS[1][:m, :n])
            qctr = [0]
            if g >= 0:
                for c in range(4):
                    def ev(ps, c=c):
                        s = qctr[0] % 2
                        qctr[0] += 1
                        fw.op("act", lambda: A.copy(out=qk_o[s][:, :n], in_=ps), reads=["ps1"], writes=[f"qko{s}"])
                        fw.dma(f"qko{s}", QT[c * 128:(c + 1) * 128, g * 512:(g + 1) * 512], qk_o[s][:, :n],
                               reads=[f"qko{s}"], writes=["QT"], q="pool")
                    fproj(c * 128, 128, ev)
            kcol0 = 0 if g < 0 else NMETA + g * 512
            for c in range(4):
                def ev(ps, c=c):
                    s = qctr[0] % 2
                    qctr[0] += 1
                    fw.op("dve", lambda: V.tensor_copy(out=qk_o[s][:, :n], in_=ps), reads=["ps1"], writes=[f"qko{s}"])
                    fw.dma(f"qko{s}", KT[c * 128:(c + 1) * 128, kcol0:kcol0 + n], qk_o[s][:, :n],
                           reads=[f"qko{s}"], writes=["KT"], q="pool")
                fproj(512 + c * 128, 128, ev)
            for c in range(4):
                fproj(1536 + c * 64, 64, lambda ps, c=c: fw.op(
                    "act", lambda: A.copy(out=gqT[:, c, :n], in_=ps), reads=["ps1"], writes=["gqT"]))
                fproj(1792 + c * 64, 64, lambda ps, c=c: fw.op(
                    "dve", lambda: V.tensor_copy(out=gkT[:, c, :n], in_=ps), reads=["ps1"], writes=["gkT"]))
            if g >= 0:
                for c in range(4):
                    fproj(2560 + c * 128, 128, lambda ps, c=c: fw.op(
                        "act", lambda: A.activation(out=grS[:, c, :n], in_=ps, func=AF.Silu), reads=["ps1"], writes=["grS"]))
            fproj(3072, 16, lambda ps: fw.op("dve", lambda: V.tensor_copy(out=glrT[:, :n], in_=ps), reads=["ps1"], writes=["glrT"]))

            if P1STOP == 2:
                fw.barrier()
                return nc
            for tl in range(ntile):
                tsl = slice(tl * 128, tl * 128 + tn)
                tok0 = (0 if g < 0 else NMETA + g * 512 + tl * 128)
                def tproj(col0, w, evac):
                    for k in range(8):
                        fw.op("pe", lambda: P.matmul(out=PS[2][:tn, :w], lhsT=hnT[:, k, tsl], rhs=win[:, k, col0:col0 + w],
                                                     start=(k == 0), stop=(k == 7)),
                              reads=["win", "hnT"], writes=["ps2"], signal=(k == 7))
                    evac(PS[2][:tn, :w])
                vs = tile_ctr[0] % 2
                tile_ctr[0] += 1
                def ev_v(ps):
                    fw.op("act", lambda: A.copy(out=vtok[vs][:tn, :], in_=ps), reads=["ps2"], writes=[f"vtok{vs}"])
                    fw.dma(f"vtok{vs}", VT[tok0:tok0 + tn, :], vtok[vs][:tn, :], reads=[f"vtok{vs}"], writes=["VT"], q="pool")
                tproj(1024, 512, ev_v)
                tproj(2048, 512, lambda ps: fw.op("dve", lambda: V.tensor_copy(out=gv[:tn, :], in_=ps), reads=["ps2"], writes=["gv"]))
                tproj(1792, 256, lambda ps: fw.op("act", lambda: A.copy(out=gkt[:tn, :], in_=ps), reads=["ps2"], writes=["gkt"]))
                if P1STOP == 3:
                    fw.barrier()
                    return nc
                fw.op("pe", lambda: P.matmul(out=PS[3][:tn, 0:256], lhsT=glrT[:, tsl], rhs=wgu[:, :], start=True, stop=False),
                      reads=["glrT", "wgu"], writes=["ps3a"], signal=False)
                fw.op("pe", lambda: P.matmul(out=PS[3][:tn, 0:256], lhsT=ones1[:, :tn], rhs=bgu[:, :], start=False, stop=True),
                      reads=["ones1", "bgu"], writes=["ps3a"])
                fw.op("act", lambda: A.activation(out=spl[:tn, :], in_=PS[3][:tn, 0:256], func=AF.Exp, scale=-1.0),
                      reads=["ps3a"], writes=["spl"])
                fw.op("act", lambda: A.activation(out=spl[:tn, :], in_=spl[:tn, :], func=AF.Ln, bias=1.0),
                      reads=["spl"], writes=["spl"])
                for c in range(4):
                    fw.op("pe", lambda: P.matmul(out=PS[4][:64, c * 128:c * 128 + tn], lhsT=spl[:tn, c * 64:(c + 1) * 64],
                                                 rhs=triI[:tn, :tn], start=True, stop=True),
                          reads=["spl", "triI"], writes=["ps4"], signal=(c == 3))
                fw.op("pe", lambda: P.matmul(out=PS[3][:tn, 256:512], lhsT=strA[:tn, :tn], rhs=spl[:tn, :], start=True, stop=True),
                      reads=["spl", "strA"], writes=["ps3b"])
                ps4v = PS[4][:64, :].rearrange("p (c t) -> p c t", c=4)[:, :, :tn]
                fw.op("act", lambda: A.activation(out=eT[:, :, :tn], in_=ps4v, func=AF.Exp), reads=["ps4"], writes=["eT"])
                fw.op("act", lambda: A.activation(out=enT[:, :, :tn], in_=ps4v, func=AF.Exp, scale=-1.0), reads=["ps4"], writes=["enT"])
                fw.op("act", lambda: A.activation(out=ebd[:tn, :], in_=PS[3][:tn, 256:512], func=AF.Exp), reads=["ps3b"], writes=["ebd"])
                if P1STOP == 4:
                    fw.barrier()
                    return nc
                fw.op("dve", lambda: V.scalar_tensor_tensor(out=qtl[:, :, :tn], in0=gqT[:, :, tsl], scalar=0.125, in1=eT[:, :, :tn],
                                                            op0=ALU.mult, op1=ALU.mult),
                      reads=["gqT", "eT"], writes=["qtl"])
                fw.op("pool", lambda: G.tensor_tensor(out=ktl[:, :, :tn], in0=gkT[:, :, tsl], in1=enT[:, :, :tn], op=ALU.mult),
                      reads=["gkT", "enT"], writes=["ktl"])
                fw.op("dve", lambda: V.tensor_tensor(out=khat[:tn, :], in0=gkt[:tn, :], in1=ebd[:tn, :], op=ALU.mult),
                      reads=["gkt", "ebd"], writes=["khat"])
                if g >= 0:
                    for h in range(4):
                        fw.op("pe", lambda: P.matmul(out=PS[5][:, h * 128:(h + 1) * 128], lhsT=ktl[:, h, :],
                                                     rhs=qtl[:, h, :], start=True, stop=True),
                              reads=["ktl", "qtl"], writes=["ps5"], signal=(h == 3))
                    fw.op("dve", lambda: V.tensor_tensor(out=attn[:, :, :], in0=PS[5][:, :].rearrange("p (h t) -> p h t", h=4),
                                                         in1=m01[:, :].unsqueeze(1).to_broadcast([128, 4, 128]), op=ALU.mult),
                          reads=["ps5", "m01"], writes=["attn"])
                    for h in range(4):
                        fw.op("pe", lambda: P.matmul(out=PS[6][:, h * 128:(h + 1) * 128], lhsT=gv[:, h * 128:(h + 1) * 128],
                                                     rhs=attn[:, h, :], start=True, stop=False),
                              reads=["gv", "attn"], writes=["ps6"], signal=False)
                        fw.op("pe", lambda: P.matmul(out=PS[6][:, h * 128:(h + 1) * 128], lhsT=stb[:, h, :],
                                                     rhs=qtl[:, h, :], start=False, stop=True),
                              reads=["stb", "qtl"], writes=["ps6"], signal=(h == 3))
                if P1STOP == 5:
                    fw.barrier()
                    return nc
                for h in range(4):
                    fw.op("pe", lambda: P.matmul(out=PS[7][:64, h * 128:(h + 1) * 128], lhsT=khat[:tn, h * 64:(h + 1) * 64],
                                                 rhs=gv[:tn, h * 128:(h + 1) * 128], start=True, stop=True),
                          reads=["khat", "gv"], writes=["ps7"], signal=(h == 3))
                for h in range(4):
                    fw.op("dve", lambda: V.scalar_tensor_tensor(
                        out=state[:, h, :], in0=state[:, h, :], scalar=eT[:, h, tn - 1:tn],
                        in1=PS[7][:64, h * 128:(h + 1) * 128],
                        op0=ALU.mult, op1=ALU.add), reads=["state", "eT", "ps7"], writes=["state"])
                fw.op("act", lambda: A.copy(out=stb[:, :, :], in_=state[:, :, :]), reads=["state"], writes=["stb"])
                if P1STOP == 6:
                    fw.barrier()
                    return nc
                if P1STOP == 7 and g >= 0:
                    fw.barrier()
                    return nc
                if g >= 0:
                    fw.op("act", lambda: A.activation(out=osq[:, :], in_=PS[6][:, :], func=AF.Square), reads=["ps6"], writes=["osq"])
                    fw.op("pe", lambda: P.matmul(out=PS[5][:, :], lhsT=ones_b[:, :], rhs=osq[:, :], start=True, stop=True),
                          reads=["osq", "ones_b"], writes=["ps5"])
                    fw.op("act", lambda: A.activation(out=rso[:, :], in_=PS[5][:, :], func=AF.Ln, scale=1.0 / 128, bias=epsc[:, :]),
                          reads=["ps5"], writes=["rso"])
                    fw.op("act", lambda: A.activation(out=rso[:, :], in_=rso[:, :], func=AF.Exp, scale=-0.5),
                          reads=["rso"], writes=["rso"])
                    fw.op("dve", lambda: V.tensor_tensor(out=o1[:, :], in0=PS[6][:, :], in1=rso[:, :], op=ALU.mult),
                          reads=["ps6", "rso"], writes=["o1"])
                    ms = tile_ctr[0] % 2
                    for h in range(4):
                        fw.op("dve", lambda: V.scalar_tensor_tensor(out=mixg[ms][:, h, :], in0=o1[:, h * 128:(h + 1) * 128],
                                                                    scalar=ggla[:, h:h + 1], in1=grS[:, h, tsl],
                                                                    op0=ALU.mult, op1=ALU.mult),
                              reads=["o1", "ggla", "grS"], writes=[f"mixg{ms}"])
                    t0 = g * 512 + tl * 128
                    fw.dma(f"mixg{ms}", MIXT[512:1024, t0:t0 + 128].rearrange("(h e) t -> e h t", h=4), mixg[ms][:, :, :],
                           reads=[f"mixg{ms}"], writes=["MIXT"], q="pool")
                next(sg, None)
                next(sg, None)
        for _ in sg:
            pass
        fw.barrier()

    if STOP < 2:
        return nc
    with ExitStack() as st:
        kts = [sb(st, f"kts{i}", [128, L], BF16) for i in range(2)]
        vts = [sb(st, f"vts{i}", [128, NT, 128], BF16) for i in range(2)]
        vmeta = [sb(st, f"vmeta{i}", [16, 128], BF16) for i in range(2)]
        qts = [sb(st, f"qts{i}", [128, 512], BF16) for i in range(2)]
        gsb = sb(st, "gsb", [128, 4], F32)
        fw.dma("c1", gsb[:, :], sb_norm_g.rearrange("o (c p) -> p (o c)", p=128), writes=["gsb"], allow_slow_non_contiguous=True)
        triK = sb(st, "triK", [128, 128], BF16)
        fw.op("dve", lambda: V.tensor_single_scalar(out=triK[:, :], in_=iot[:, :], scalar=0.0, op=ALU.is_le),
              reads=["iot"], writes=["triK"])
        ones2 = sb(st, "ones2", [128, 128], BF16)
        fw.op("dve", lambda: V.memset(ones2[:, :], 0.0), writes=["ones2"])
        fw.op("dve", lambda: V.memset(ones2[0:64, 0:64], 1.0), writes=["ones2"])
        fw.op("dve", lambda: V.memset(ones2[64:128, 64:128], 1.0), writes=["ones2"])
        dmask = sb(st, "dmask", [128, 4, 512], BF16)
        iot4 = sb(st, "iot4", [128, 512], F32)
        fw.op("pool", lambda: G.iota(iot4[:, :], pattern=[[1, 512]], base=0, channel_multiplier=-1,
                                     allow_small_or_imprecise_dtypes=True), writes=["iot4"])
        for a_ in range(4):
            fw.op("dve", lambda: V.tensor_single_scalar(out=dmask[:, a_, :], in_=iot4[:, :], scalar=float(128 * a_), op=ALU.is_gt),
                  reads=["iot4"], writes=["dmask"])
        NB = 4
        ee = [[sb(st, f"ee{u}_{i}", [128, 512], BF16) for i in range(NB)] for u in range(2)]
        spb = [[sb(st, f"spb{u}_{i}", [128, 512], BF16) for i in range(NB)] for u in range(2)]
        ww = [[sb(st, f"ww{u}_{i}", [128, 512], BF16) for i in range(NB)] for u in range(2)]
        aa = [[sb(st, f"aa{u}_{i}", [128, 512], BF16) for i in range(NB)] for u in range(2)]
        Sacc = [[sb(st, f"Sacc{u}_{i}", [128, 512], BF16) for i in range(2)] for u in range(2)]
        osq2 = sb(st, "osq2", [128, 512], BF16)
        rs2 = sb(st, "rs2", [128, 512], F32)
        mo = [sb(st, f"mo{i}", [128, 512], BF16) for i in range(2)]

        tctr = [0]
        gctr = [0]

        def load_kv(hp_):
            hs_ = hp_ % 2
            fw.dma(f"kts{hs_}", kts[hs_][:, :], KT[hp_ * 128:(hp_ + 1) * 128, :], writes=[f"kts{hs_}"])
            fw.dma(f"vts{hs_}", vts[hs_][:, :, :], VT[NMETA:L, hp_ * 128:(hp_ + 1) * 128].rearrange("(j p) d -> p j d", p=128),
                   writes=[f"vts{hs_}"])
            fw.dma(f"vmeta{hs_}", vmeta[hs_][:, :], VT[0:NMETA, hp_ * 128:(hp_ + 1) * 128], writes=[f"vmeta{hs_}"])

        def load_q(idx):
            hp_, g_ = divmod(idx, NG)
            gs_ = idx % 2
            fw.dma(f"qts{gs_}", qts[gs_][:, :], QT[hp_ * 128:(hp_ + 1) * 128, g_ * 512:(g_ + 1) * 512], writes=[f"qts{gs_}"])

        load_kv(0)
        load_q(0)
        for hp in range(4):
            hs = hp % 2
            for g in range(NG):
                gs = gctr[0] % 2
                gctr[0] += 1
                if g == 0 and hp + 1 < 4:
                    load_kv(hp + 1)
                if hp * NG + g + 1 < 4 * NG:
                    load_q(hp * NG + g + 1)
                blocks = [(4 * g + a_, a_) for a_ in (3, 2, 1, 0)] + [(j, None) for j in range(4 * g - 1, -1, -1)] + [(-1, None)]
                nb = len(blocks)

                def stage_z(i):
                    j, a_ = blocks[i]
                    t = tctr[0] + i
                    kn = 16 if j < 0 else 128
                    kc0 = 0 if j < 0 else NMETA + j * 128
                    s3 = t % NB
                    for u in range(2):
                        zb = 2 * u + t % 2
                        po = 64 * u
                        fw.op("pe", lambda: P.matmul(out=PS[zb][:kn, :], lhsT=kts[hs][po:po + 64, kc0:kc0 + kn],
                                                     rhs=qts[gs][po:po + 64, :], start=True, stop=True),
                              reads=[f"kts{hs}", f"qts{gs}"], writes=[f"ps{zb}"])
                    for u in range(2):
                        zb = 2 * u + t % 2
                        fw.op("act", lambda: A.activation(out=ee[u][s3][:kn, :], in_=PS[zb][:kn, :], func=AF.Exp, scale=0.125),
                              reads=[f"ps{zb}"], writes=[f"ee{u}_{s3}"])
                        fw.op("act", lambda: A.activation(out=spb[u][s3][:kn, :], in_=ee[u][s3][:kn, :], func=AF.Ln, bias=1.0),
                              reads=[f"ee{u}_{s3}"], writes=[f"spb{u}_{s3}"])
                        if a_ is not None:
                            fw.op("pool", lambda: G.tensor_tensor(out=spb[u][s3][:, :], in0=spb[u][s3][:, :], in1=dmask[:, a_, :],
                                                                  op=ALU.mult),
                                  reads=[f"spb{u}_{s3}", "dmask"], writes=[f"spb{u}_{s3}"])
                            fw.op("pool", lambda: G.tensor_tensor(out=ee[u][s3][:, :], in0=ee[u][s3][:, :], in1=dmask[:, a_, :],
                                                                  op=ALU.mult),
                                  reads=[f"ee{u}_{s3}", "dmask"], writes=[f"ee{u}_{s3}"])

                def stage_c(i):
                    j, a_ = blocks[i]
                    t = tctr[0] + i
                    s3 = t % NB
                    kn = 16 if j < 0 else 128
                    first = (i == 0)
                    for u in range(2):
                        cb = 4 + u
                        fw.op("pe", lambda: P.matmul(out=PS[cb][:kn, :], lhsT=triK[:kn, :kn], rhs=spb[u][s3][:kn, :],
                                                     start=True, stop=first),
                              reads=["triK", f"spb{u}_{s3}"], writes=[f"ps{cb}"], signal=first)
                        if not first:
                            fw.op("pe", lambda: P.matmul(out=PS[cb][:kn, :], lhsT=ones_b[:, :kn], rhs=Sacc[u][gs][:, :],
                                                         start=False, stop=True),
                                  reads=["ones_b", f"Sacc{u}_{gs}"], writes=[f"ps{cb}"])
                        fw.op("act", lambda: A.activation(out=ww[u][s3][:kn, :], in_=PS[cb][:kn, :], func=AF.Exp, scale=-1.0),
                              reads=[f"ps{cb}"], writes=[f"ww{u}_{s3}"])
                        fw.op("dve", lambda: V.tensor_tensor(out=aa[u][s3][:kn, :], in0=ee[u][s3][:kn, :], in1=ww[u][s3][:kn, :],
                                                             op=ALU.mult),
                              reads=[f"ee{u}_{s3}", f"ww{u}_{s3}"], writes=[f"aa{u}_{s3}"])
                        if j >= 0:
                            if first:
                                fw.op("dve", lambda: V.tensor_copy(out=Sacc[u][gs][:, :], in_=spb[u][s3][:, :]),
                                      reads=[f"spb{u}_{s3}"], writes=[f"Sacc{u}_{gs}"])
                            else:
                                fw.op("dve", lambda: V.tensor_tensor(out=Sacc[u][gs][:, :], in0=Sacc[u][gs][:, :],
                                                                     in1=spb[u][s3][:, :], op=ALU.add),
                                      reads=[f"spb{u}_{s3}", f"Sacc{u}_{gs}"], writes=[f"Sacc{u}_{gs}"])

                def stage_av(i):
                    j, a_ = blocks[i]
                    t = tctr[0] + i
                    s3 = t % NB
                    kn = 16 if j < 0 else 128
                    for u in range(2):
                        po = 64 * u
                        lhs = vmeta[hs][:, po:po + 64] if j < 0 else vts[hs][:, j, po:po + 64]
                        fw.op("pe", lambda: P.matmul(out=PS[6][po:po + 64, :], lhsT=lhs, rhs=aa[u][s3][:kn, :],
                                                     start=(i == 0), stop=(i == nb - 1), skip_group_check=True),
                              reads=[f"vts{hs}", f"vmeta{hs}", f"aa{u}_{s3}"], writes=["ps6"], signal=(u == 1 and i == nb - 1))

                stage_z(0)
                if nb > 1:
                    stage_z(1)
                stage_c(0)
                for i in range(nb):
                    if i + 2 < nb:
                        stage_z(i + 2)
                    if i + 1 < nb:
                        stage_c(i + 1)
                    stage_av(i)
                tctr[0] += nb
                fw.op("act", lambda: A.activation(out=osq2[:, :], in_=PS[6][:, :], func=AF.Square), reads=["ps6"], writes=["osq2"])
                fw.op("pe", lambda: P.matmul(out=PS[7][:, :], lhsT=ones2[:, :], rhs=osq2[:, :], start=True, stop=True),
                      reads=["osq2", "ones2"], writes=["ps7"])
                fw.op("act", lambda: A.activation(out=rs2[:, :], in_=PS[7][:, :], func=AF.Ln, scale=1.0 / 64, bias=epsc[:, :]),
                      reads=["ps7"], writes=["rs2"])
                fw.op("act", lambda: A.activation(out=rs2[:, :], in_=rs2[:, :], func=AF.Exp, scale=-0.5), reads=["rs2"], writes=["rs2"])
                fw.op("dve", lambda: V.scalar_tensor_tensor(out=mo[gs][:, :], in0=PS[6][:, :], scalar=gsb[:, hp:hp + 1], in1=rs2[:, :],
                                                            op0=ALU.mult, op1=ALU.mult),
                      reads=["ps6", "gsb", "rs2"], writes=[f"mo{gs}"])
                fw.dma(f"mo{gs}", MIXT[hp * 128:(hp + 1) * 128, g * 512:(g + 1) * 512], mo[gs][:, :], reads=[f"mo{gs}"], writes=["MIXT"])
        fw.barrier()

    if STOP < 3:
        return nc
    TG = 256
    with ExitStack() as st:
        wout = sb(st, "wout", [128, 8, D], BF16)
        wq = sb(st, "wq", [128, 8, D], BF16)
        g2b = sb(st, "g2b", [128, D], F32)
        gfb = sb(st, "gfb", [128, D], F32)
        fw.dma("c3", g2b[:, :], norm2_g[0:1, :].partition_broadcast(128).rearrange("p o n -> p (o n)"), writes=["g2b"])
        fw.dma("c4", gfb[:, :], norm_f_g[0:1, :].partition_broadcast(128).rearrange("p o n -> p (o n)"), writes=["gfb"])
        iota_b = sb(st, "iota_b", [128, 16, 128], BF16)
        fw.op("dve", lambda: V.tensor_copy(out=iota_b[:, :, :], in_=iotc[:, :].unsqueeze(1).to_broadcast([128, 16, 128])),
              reads=["iotc"], writes=["iota_b"])
        iota16 = iotc[:, 0:16]
        skb = sb(st, "skb", [128, 8, 256], BF16)
        with ExitStack() as st2:
            wstg = [sb(st2, f"wstg3{i}", [128, D], F32) for i in range(2)]
            for wi, (src, dst, nm) in enumerate(((w_out, wout, "wout"), (w_peer_q, wq, "wq"))):
                for k in range(8):
                    s = k % 2
                    fw.dma(f"wstg3{s}", wstg[s][:, :], src[k * 128:(k + 1) * 128, :], writes=[f"wstg3{s}"])
                    if k % 2 == 0:
                        fw.op("act", lambda: A.copy(out=dst[:, k, :], in_=wstg[s][:, :]), reads=[f"wstg3{s}"], writes=[nm])
                    else:
                        fw.op("dve", lambda: V.tensor_copy(out=dst[:, k, :], in_=wstg[s][:, :]), reads=[f"wstg3{s}"], writes=[nm])
            skst = sb(st2, "skst", [128, 8, 128], F32)
            fw.dma("c1", skst[:, :, 0:64], sub_keys1.rearrange("h k d -> k h d"), writes=["skst"], allow_slow_non_contiguous=True)
            fw.dma("c2", skst[:, :, 64:128], sub_keys2.rearrange("h k d -> k h d"), writes=["skst"], allow_slow_non_contiguous=True)
            fw.op("dve", lambda: V.memset(skb[:, :, :], 0.0), writes=["skb"])
            for h in range(8):
                fw.op("pe", lambda: P.transpose(out=PS[0][:, 0:128], in_=skst[:, h, :], identity=ident_f[:, :]),
                      reads=["skst"], writes=["ps0"])
                fw.op("dve", lambda: V.tensor_copy(out=skb[0:64, h, 0:128], in_=PS[0][0:64, 0:128]), reads=["ps0"], writes=["skb"])
                fw.op("dve", lambda: V.tensor_copy(out=skb[64:128, h, 128:256], in_=PS[0][64:128, 0:128]), reads=["ps0"], writes=["skb"])
        fw.barrier()

        mixs = sb(st, "mixs", [128, 8, TG], BF16)
        xs3 = sb(st, "xs3", [128, D], F32)
        h1 = sb(st, "h1", [128, 2, 2, D], F32)
        ss = sb(st, "ss3", [128, 1], F32)
        rs = sb(st, "rs3", [128, 1], F32)
        ss4 = sb(st, "ss4", [128, 1], F32)
        rs4 = sb(st, "rs4", [128, 1], F32)
        hn2 = sb(st, "hn2", [128, D], BF16)
        hn2T = sb(st, "hn2T", [128, 2, 8, TG], BF16)
        qpT = mixs
        ssb = sb(st, "ssb", [128, 4, 256], F32)
        wk = sb(st, "wk", [128, 256], F32)
        vv = sb(st, "vv", [128, 8, 2, 16], F32)
        ix = sb(st, "ix", [128, 8, 2, 16], U32)
        ixf = sb(st, "ixf", [128, 8, 2, 16], F32)
        cand = sb(st, "cand", [128, 8, 16, 16], F32)
        cc = sb(st, "cc", [128, 8, 16], F32)
        pos = sb(st, "pos", [128, 8, 16], U32)
        pa = sb(st, "pa", [128, 8, 16], U32)
        pb = sb(st, "pb", [128, 8, 16], U32)
        paf = sb(st, "paf", [128, 8, 16], F32)
        pbf = sb(st, "pbf", [128, 8, 16], F32)
        sel = cand
        Isel = sb(st, "Isel", [128, 128], F32)
        Jsel = sb(st, "Jsel", [128, 128], F32)
        gte = sb(st, "gte", [128, 8, 16], F32)
        zz = sb(st, "zz", [128, 8], F32)
        gate = sb(st, "gate", [128, 128], F32)
        ITb = sb(st, "ITb", [128, 2, TG], BF16)
        JTb = sb(st, "JTb", [128, 2, TG], BF16)
        GTb = sb(st, "GTb", [128, 2, TG], BF16)
        OA = [sb(st, f"OA{i}", [128, 16, 128], BF16) for i in range(2)]
        OB = [sb(st, f"OB{i}", [128, 16, 128], BF16) for i in range(2)]
        Gs = sb(st, "Gs", [128, 128, TG], BF16)
        utc = [sb(st, f"utc{i}", [128, 8, 128], BF16) for i in range(4)]
        vbc = [sb(st, f"vbc{i}", [128, D], BF16) for i in range(4)]
        gl = [sb(st, f"gl{i}", [128, TG], BF16) for i in range(2)]
        ga = [sb(st, f"ga{i}", [128, TG], BF16) for i in range(2)]
        h2 = xs3
        fw.barrier()

        def selection(grp):
            sl = grp % 2
            c0 = grp * TG
            fw.dma("mixs", mixs[:, :, :], MIXT[:, c0:c0 + TG].rearrange("(k p) t -> p k t", p=128), writes=["mixs"])
            for tl in range(2):
                fw.dma("xs3", xs3[:, :], x[c0 + tl * 128:c0 + (tl + 1) * 128, :], writes=["xs3"])
                for hf in range(2):
                    bk = 6 + hf
                    for k in range(8):
                        fw.op("pe", lambda: P.matmul(out=PS[bk][:, :], lhsT=mixs[:, k, tl * 128:(tl + 1) * 128],
                                                     rhs=wout[:, k, hf * 512:(hf + 1) * 512], start=(k == 0), stop=(k == 7)),
                              reads=["mixs", "wout"], writes=[f"ps{bk}"], signal=(k == 7))
                    fw.op("dve", lambda: V.tensor_tensor(out=h1[:, sl, tl, hf * 512:(hf + 1) * 512], in0=PS[bk][:, :],
                                                         in1=xs3[:, hf * 512:(hf + 1) * 512], op=ALU.add),
                          reads=[f"ps{bk}", "xs3"], writes=[f"h1_{sl}_{tl}"])
                    yield
                fw.op("act", lambda: A.activation(out=hn2[:, :], in_=h1[:, sl, tl, :], func=AF.Square, accum_out=ss[:, :]),
                      reads=[f"h1_{sl}_{tl}"], writes=["hn2", "ss3"])
                rstd_from_ss(ss, rs, D, "ss3", "rs3")
                fw.op("dve", lambda: V.scalar_tensor_tensor(out=hn2[:, :], in0=h1[:, sl, tl, :], scalar=rs[:, :], in1=g2b[:, :],
                                                            op0=ALU.mult, op1=ALU.mult),
                      reads=[f"h1_{sl}_{tl}", "rs3", "g2b"], writes=["hn2"])
                yield
                ptr = PS[6][:, :].bitcast(BF16)
                for k in range(8):
                    fw.op("pe", lambda: P.transpose(out=ptr[:, k * 128:(k + 1) * 128], in_=hn2[:, k * 128:(k + 1) * 128],
                                                    identity=ident_b[:, :]),
                          reads=["hn2"], writes=["ps6"], signal=(k == 7))
                fw.op("act", lambda: A.copy(out=hn2T[:, sl, :, tl * 128:(tl + 1) * 128], in_=ptr.rearrange("p (k t) -> p k t", k=8)),
                      reads=["ps6"], writes=[f"hn2T{sl}"])
                yield
            for h in range(8):
                bk = 6 + h % 2
                for k in range(8):
                    fw.op("pe", lambda: P.matmul(out=PS[bk][:, 0:TG], lhsT=wq[:, k, h * 128:(h + 1) * 128], rhs=hn2T[:, sl, k, :],
                                                 start=(k == 0), stop=(k == 7)),
                          reads=["wq", f"hn2T{sl}"], writes=[f"ps{bk}"], signal=(k == 7))
                if h % 2 == 0:
                    fw.op("act", lambda: A.copy(out=qpT[:, h, :], in_=PS[bk][:, 0:TG]), reads=[f"ps{bk}"], writes=["mixs"])
                else:
                    fw.op("dve", lambda: V.tensor_copy(out=qpT[:, h, :], in_=PS[bk][:, 0:TG]), reads=[f"ps{bk}"], writes=["mixs"])
                yield
            for tl in range(2):
                tsl = slice(tl * 128, (tl + 1) * 128)
                for hq in range(2):
                    for hp in range(2):
                        bk = 6 + hp
                        for hh in range(2):
                            h = hq * 4 + hp * 2 + hh
                            fw.op("pe", lambda: P.matmul(out=PS[bk][:, hh * 256:(hh + 1) * 256], lhsT=qpT[:, h, tsl], rhs=skb[:, h, :],
                                                         start=True, stop=True),
                                  reads=["mixs", "skb"], writes=[f"ps{bk}"], signal=(hh == 1))
                        fw.op("act", lambda: A.copy(out=ssb[:, hp * 2:hp * 2 + 2, :].rearrange("p a b -> p (a b)"), in_=PS[bk][:, :]),
                              reads=[f"ps{bk}"], writes=["ssb"])
                    yield
                    for h4 in range(4):
                        h = hq * 4 + h4
                        for hf in range(2):
                            src = ssb[:, h4, hf * 128:(hf + 1) * 128]
                            fw.op("dve", lambda: V.max(out=vv[:, h, hf, 0:8], in_=src), reads=["ssb"], writes=["vv"])
                            fw.op("dve", lambda: V.match_replace(out=wk[:, 0:128], in_to_replace=vv[:, h, hf, 0:8], in_values=src,
                                                                 imm_value=NEG), reads=["ssb", "vv"], writes=["wk"])
                            fw.op("dve", lambda: V.max(out=vv[:, h, hf, 8:16], in_=wk[:, 0:128]), reads=["wk"], writes=["vv"])
                            fw.op("dve", lambda: V.max_index(out=ix[:, h, hf, 0:8], in_max=vv[:, h, hf, 0:8], in_values=src),
                                  reads=["ssb", "vv"], writes=["ix"])
                            fw.op("dve", lambda: V.max_index(out=ix[:, h, hf, 8:16], in_max=vv[:, h, hf, 8:16], in_values=src),
                                  reads=["ssb", "vv"], writes=["ix"])
                            yield
                fw.op("dve", lambda: V.tensor_copy(out=ixf[:, :, :, :], in_=ix[:, :, :, :]), reads=["ix"], writes=["ixf"])
                fw.op("dve", lambda: V.tensor_tensor(out=cand[:, :, :, :], in0=vv[:, :, 0, :].unsqueeze(3).to_broadcast([128, 8, 16, 16]),
                                                     in1=vv[:, :, 1, :].unsqueeze(2).to_broadcast([128, 8, 16, 16]), op=ALU.add),
                      reads=["vv"], writes=["cand"])
                yield
                for h in range(8):
                    src = cand[:, h, :, :].rearrange("p a b -> p (a b)")
                    fw.op("dve", lambda: V.max(out=cc[:, h, 0:8], in_=src), reads=["cand"], writes=["cc"])
                    fw.op("dve", lambda: V.match_replace(out=wk[:, :], in_to_replace=cc[:, h, 0:8], in_values=src, imm_value=NEG),
                          reads=["cand", "cc"], writes=["wk"])
                    fw.op("dve", lambda: V.max(out=cc[:, h, 8:16], in_=wk[:, :]), reads=["wk"], writes=["cc"])
                    fw.op("dve", lambda: V.max_index(out=pos[:, h, 0:8], in_max=cc[:, h, 0:8], in_values=src), reads=["cand", "cc"], writes=["pos"])
                    fw.op("dve", lambda: V.max_index(out=pos[:, h, 8:16], in_max=cc[:, h, 8:16], in_values=src), reads=["cand", "cc"], writes=["pos"])
                    yield
                fw.op("dve", lambda: V.tensor_single_scalar(out=pa[:, :, :], in_=pos[:, :, :], scalar=4, op=ALU.logical_shift_right),
                      reads=["pos"], writes=["pa"])
                fw.op("dve", lambda: V.tensor_single_scalar(out=pb[:, :, :], in_=pos[:, :, :], scalar=15, op=ALU.bitwise_and),
                      reads=["pos"], writes=["pb"])
                fw.op("dve", lambda: V.tensor_copy(out=paf[:, :, :], in_=pa[:, :, :]), reads=["pa"], writes=["paf"])
                fw.op("dve", lambda: V.tensor_copy(out=pbf[:, :, :], in_=pb[:, :, :]), reads=["pb"], writes=["pbf"])
                yield
                for (pf, hf, dst, nm) in ((paf, 0, Isel, "Isel"), (pbf, 1, Jsel, "Jsel")):
                    fw.op("dve", lambda: V.tensor_tensor(out=sel[:, :, :, :],
                                                         in0=iota16.unsqueeze(1).unsqueeze(1).to_broadcast([128, 8, 16, 16]),
                                                         in1=pf[:, :, :].unsqueeze(3).to_broadcast([128, 8, 16, 16]), op=ALU.is_equal),
                          reads=["iotc", "paf", "pbf"], writes=["cand"])
                    yield
                    fw.op("dve", lambda: V.tensor_tensor(out=sel[:, :, :, :], in0=sel[:, :, :, :],
                                                         in1=ixf[:, :, hf, :].unsqueeze(2).to_broadcast([128, 8, 16, 16]), op=ALU.mult),
                          reads=["cand", "ixf"], writes=["cand"])
                    yield
                    fw.op("dve", lambda: V.tensor_reduce(out=dst[:, :], in_=sel[:, :, :, :].rearrange("p h r a -> p (h r) a"),
                                                         axis=AX.X, op=ALU.add),
                          reads=["cand"], writes=[nm])
                    yield
                fw.op("dve", lambda: V.tensor_tensor(out=gte[:, :, :], in0=cc[:, :, :], in1=cc[:, :, 0:1].to_broadcast([128, 8, 16]),
                                                     op=ALU.subtract), reads=["cc"], writes=["gte"])
                fw.op("act", lambda: A.activation(out=gte[:, :, :], in_=gte[:, :, :], func=AF.Exp), reads=["gte"], writes=["gte"])
                fw.op("dve", lambda: V.tensor_reduce(out=zz[:, :], in_=gte[:, :, :], axis=AX.X, op=ALU.add), reads=["gte"], writes=["zz"])
                fw.op("dve", lambda: V.reciprocal(out=zz[:, :], in_=zz[:, :]), reads=["zz"], writes=["zz"])
                fw.op("dve", lambda: V.tensor_tensor(out=gate[:, :].rearrange("p (h r) -> p h r", h=8), in0=gte[:, :, :],
                                                     in1=zz[:, :].unsqueeze(2).to_broadcast([128, 8, 16]), op=ALU.mult),
                      reads=["gte", "zz"], writes=["gate"])
                yield
                for qi, (srcT, nm, dstT) in enumerate(((Isel, "Isel", ITb), (Jsel, "Jsel", JTb), (gate, "gate", GTb))):
                    fw.op("pe", lambda: P.transpose(out=PS[7][:, qi * 128:(qi + 1) * 128], in_=srcT[:, :], identity=ident_f[:, :]),
                          reads=[nm], writes=["ps7"])
                    fw.op("act", lambda: A.copy(out=dstT[:, sl, tsl], in_=PS[7][:, qi * 128:(qi + 1) * 128]), reads=["ps7"],
                          writes=[f"{nm}T{sl}"])
                yield

        def gbuild_half(grp, half):
            sl = grp % 2
            gk = "GsLo" if half == 0 else "GsHi"
            i0 = half * 64

            def prep(sbt):
                o = sbt % 2
                t0 = sbt * 16
                fw.op("dve", lambda: V.tensor_tensor(out=OB[o][:, :, :], in0=iota_b[:, :, :],
                                                     in1=JTb[:, sl, t0:t0 + 16].unsqueeze(2).to_broadcast([128, 16, 128]), op=ALU.is_equal),
                      reads=["iota_b", f"JselT{sl}"], writes=[f"OB{o}"])
                fw.op("dve", lambda: V.tensor_tensor(out=OA[o][:, :, 0:64], in0=iota_b[:, :, i0:i0 + 64],
                                                     in1=ITb[:, sl, t0:t0 + 16].unsqueeze(2).to_broadcast([128, 16, 64]), op=ALU.is_equal),
                      reads=["iota_b", f"IselT{sl}"], writes=[f"OA{o}"])
                fw.op("pool", lambda: G.tensor_tensor(out=OA[o][:, :, 0:64], in0=OA[o][:, :, 0:64],
                                                      in1=GTb[:, sl, t0:t0 + 16].unsqueeze(2).to_broadcast([128, 16, 64]), op=ALU.mult),
                      reads=[f"OA{o}", f"gateT{sl}"], writes=[f"OA{o}"])

            def mm(sbt):
                o = sbt % 2
                t0 = sbt * 16
                for q in range(2):
                    bk = 6 + q
                    for tt in range(8):
                        ti = q * 8 + tt
                        fw.op("pe", lambda: P.matmul(out=PS[bk][:, tt * 64:(tt + 1) * 64], lhsT=OB[o][:, ti, :], rhs=OA[o][:, ti, 0:64],
                                                     start=True, stop=True),
                              reads=[f"OA{o}", f"OB{o}"], writes=[f"ps{bk}"], signal=(tt == 7))
                    tq = t0 + q * 8
                    src = PS[bk][:, :].rearrange("p (t i) -> p i t", t=8)
                    fw.op("act", lambda: A.copy(out=Gs[:, i0:i0 + 64, tq:tq + 8], in_=src), reads=[f"ps{bk}"], writes=[gk])

            nsb = TG // 16
            prep(0)
            yield
            for sbt in range(nsb):
                if sbt + 1 < nsb:
                    prep(sbt + 1)
                    yield
                mm(sbt)
                yield

        def eloop(grp, hi_gen, sel_gen, lo_gen, preloaded, preload_next):
            sl = grp % 2

            def ld(i):
                s = i % 4
                fw.dma(f"utc{s}", utc[s][:, :, :], UT[i], writes=[f"utc{s}"])
                fw.dma(f"vbc{s}", vbc[s][:, :], VB[i * 128:(i + 1) * 128, :], writes=[f"vbc{s}"])

            def st_u(i):
                s = i % 4
                hb = i % 2
                for k in range(8):
                    fw.op("pe", lambda: P.matmul(out=PS[4 + hb][:, 0:TG], lhsT=utc[s][:, k, :], rhs=hn2T[:, sl, k, :],
                                                 start=(k == 0), stop=(k == 7)),
                          reads=[f"utc{s}", f"hn2T{sl}"], writes=[f"ps{4 + hb}"], signal=(k == 7))
                fw.op("act", lambda: A.activation(out=gl[hb][:, :], in_=PS[4 + hb][:, 0:TG], func=AF.Gelu),
                      reads=[f"ps{4 + hb}"], writes=[f"gl{hb}"])
                fw.op("pool", lambda: G.tensor_tensor(out=ga[hb][:, :], in0=gl[hb][:, :], in1=Gs[:, i, :], op=ALU.mult),
                      reads=[f"gl{hb}", "GsLo" if i < 64 else "GsHi"], writes=[f"ga{hb}"])

            def st_v(i):
                s = i % 4
                hb = i % 2
                for tl in range(2):
                    for hf in range(2):
                        bk = 2 * tl + hf
                        fw.op("pe", lambda: P.matmul(out=PS[bk][:, :], lhsT=ga[hb][:, tl * 128:(tl + 1) * 128],
                                                     rhs=vbc[s][:, hf * 512:(hf + 1) * 512], start=(i == 0), stop=(i == 127)),
                              reads=[f"ga{hb}", f"vbc{s}"], writes=[f"ps{bk}"], signal=(tl == 1 and hf == 1))

            def drain(gen):
                if gen is not None:
                    for _ in gen:
                        pass

            def step(gen):
                if gen is None:
                    return True
                try:
                    next(gen)
                    return False
                except StopIteration:
                    return True

            hi_done = hi_gen is None
            sel_done = sel_gen is None
            if not preloaded:
                ld(0)
                ld(1)
                ld(2)
            st_u(0)
            for i in range(128):
                if i + 3 < 128:
                    ld(i + 3)
                elif preload_next:
                    ld(i + 3 - 128)
                if i + 1 < 128:
                    if i + 1 == 64 and not hi_done:
                        drain(hi_gen)
                        hi_done = True
                    st_u(i + 1)
                st_v(i)
                if i >= 1:
                    if not hi_done:
                        hi_done = step(hi_gen)
                    if not sel_done:
                        sel_done = step(sel_gen)
                    elif i >= 63 and lo_gen is not None:
                        step(lo_gen)
            drain(sel_gen)
            drain(lo_gen)

        def epilogue(grp):
            sl = grp % 2
            c0 = grp * TG
            for tl in range(2):
                for hf in range(2):
                    bk = 2 * tl + hf
                    fw.op("dve", lambda: V.tensor_tensor(out=h2[:, hf * 512:(hf + 1) * 512], in0=PS[bk][:, :],
                                                         in1=h1[:, sl, tl, hf * 512:(hf + 1) * 512], op=ALU.add),
                          reads=[f"ps{bk}", f"h1_{sl}_{tl}"], writes=["xs3"])
                fw.op("act", lambda: A.activation(out=OA[0][:, 0:8, :].rearrange("p a b -> p (a b)"), in_=h2[:, :], func=AF.Square,
                                                  accum_out=ss4[:, :]),
                      reads=["xs3"], writes=["OA0", "ss4"])
                rstd_from_ss(ss4, rs4, D, "ss4", "rs4")
                fw.op("dve", lambda: V.scalar_tensor_tensor(out=h2[:, :], in0=h2[:, :], scalar=rs4[:, :], in1=gfb[:, :],
                                                            op0=ALU.mult, op1=ALU.mult),
                      reads=["xs3", "rs4", "gfb"], writes=["xs3"])
                fw.dma("xs3", out[c0 + tl * 128:c0 + (tl + 1) * 128, :], h2[:, :], reads=["xs3"], writes=["out"])

        ngrp = NR // TG
        for _ in selection(0):
            pass
        for _ in gbuild_half(0, 0):
            pass
        for _ in gbuild_half(0, 1):
            pass
        for grp in range(ngrp):
            last = grp + 1 >= ngrp
            eloop(grp,
                  gbuild_half(grp, 1) if grp > 0 else None,
                  None if last else selection(grp + 1),
                  None if last else gbuild_half(grp + 1, 0),
                  grp > 0, not last)
            epilogue(grp)
        fw.barrier()
    glob.close()
    return nc


_CACHE = {}


def kernel(**inputs):
    xf = np.ascontiguousarray(np.asarray(inputs["x"], dtype=np.float32))
    B, NR, _ = xf.shape
    assert B == 8 and NR % 512 == 0
    if NR not in _CACHE:
        _CACHE[NR] = build(NR)
    nc = _CACHE[NR]
    f = lambda k: np.ascontiguousarray(np.asarray(inputs[k], dtype=np.float32))
    shared = {
        "meta_tokens": f("meta_tokens"),
        "norm1_g": f("norm1_g").reshape(1, D),
        "w_in": f("w_in").reshape(D, INC),
        "w_gate_up": f("w_gate_up").reshape(16, 256),
        "b_gate_up": f("b_gate_up").reshape(1, 256),
        "sb_norm_g": f("sb_norm_g").reshape(1, 512),
        "gla_norm_g": f("gla_norm_g").reshape(1, 512),
        "w_out": f("w_out").reshape(D, D),
        "norm2_g": f("norm2_g").reshape(1, D),
        "w_peer_q": f("w_peer_q").reshape(D, D),
        "sub_keys1": f("sub_keys1").reshape(8, 128, 64),
        "sub_keys2": f("sub_keys2").reshape(8, 128, 64),
        "expert_u": f("expert_u").reshape(16384, D),
        "expert_v": f("expert_v").reshape(16384, D),
        "norm_f_g": f("norm_f_g").reshape(1, D),
    }
    in_maps = [dict(shared, x=xf[c]) for c in range(8)]
    res = run_bass_kernel_spmd(nc, in_maps, core_ids=list(range(8)))
    return np.stack([np.asarray(res.results[c]["out"], dtype=np.float32).reshape(NR, D) for c in range(8)], axis=0)
```
